# Optimizing a Trainium2 kernel written in Bass

```python
import math
import jax
import jax.numpy as jnp
from jax import lax
import numpy as np


D_MODEL = 1024
BATCH = 8
SEQ = 4096
DEPTH = 1

HEAD_DIM = 64
N_HEADS_SB = 8
N_HEADS_MB = 8
D_SB = N_HEADS_SB * HEAD_DIM
D_MB = N_HEADS_MB * HEAD_DIM
D_MIX = D_SB + D_MB
D_PLE = 256
SB_QBLOCK = 128
MOBA_BLOCK = 256
MOBA_TOPK = 3
MOBA_QCHUNK = 16
RMS_EPS = 1e-6

kernel_name = "hybrid_stickbreak_moba_layer"


def rmsnorm(x, g):
    xf = x.astype(jnp.float32)
    y = xf * lax.rsqrt(jnp.mean(xf * xf, axis=-1, keepdims=True) + RMS_EPS)
    return (y * g.astype(jnp.float32)).astype(x.dtype)


def split_heads(t, n_heads):
    b, s, _ = t.shape
    return t.reshape(b, s, n_heads, HEAD_DIM).transpose(0, 2, 1, 3)


def merge_heads(t):
    b, h, s, d = t.shape
    return t.transpose(0, 2, 1, 3).reshape(b, s, h * d)


def alibi_slopes(n_heads):
    return jnp.asarray(np.power(2.0, -8.0 * np.arange(1, n_heads + 1) / n_heads).astype(np.float32))


def stick_breaking_attention(q, k, v):
    b, h, s, d = q.shape
    scale = 1.0 / math.sqrt(d)
    qf = q.astype(jnp.float32)
    kf = k.astype(jnp.float32)
    vf = v.astype(jnp.float32)
    outs = []
    for i in range(s // SB_QBLOCK):
        t0 = i * SB_QBLOCK
        t1 = t0 + SB_QBLOCK
        z = jnp.einsum("bhqd,bhkd->bhqk", qf[:, :, t0:t1], kf[:, :, :t1]) * scale
        past = jnp.arange(t1)[None, :] < (t0 + jnp.arange(SB_QBLOCK))[:, None]
        log_keep = jnp.where(past, jax.nn.log_sigmoid(-z), 0.0)
        log_after = lax.cumsum(log_keep, axis=3, reverse=True) - log_keep
        w = jnp.where(past, jnp.exp(jax.nn.log_sigmoid(z) + log_after), 0.0)
        outs.append(jnp.einsum("bhqk,bhkd->bhqd", w, vf[:, :, :t1]))
    return jnp.concatenate(outs, axis=2).astype(v.dtype)


def moba_attention(q, k, v, slopes):
    b, h, s, d = q.shape
    scale = 1.0 / math.sqrt(d)
    n_blk = -(-s // MOBA_BLOCK)
    s_pad = n_blk * MOBA_BLOCK
    pad = ((0, 0), (0, 0), (0, s_pad - s), (0, 0))
    qf = jnp.pad(q.astype(jnp.float32), pad)
    kf = jnp.pad(k.astype(jnp.float32), pad)
    vf = jnp.pad(v.astype(jnp.float32), pad)
    kb = kf.reshape(b, h, n_blk, MOBA_BLOCK, d)
    vb = vf.reshape(b, h, n_blk, MOBA_BLOCK, d)

    k_mean = kb.mean(axis=3)
    gate = jnp.einsum("bhsd,bhnd->bhsn", qf, k_mean)
    q_blk = jnp.arange(s_pad) // MOBA_BLOCK
    fully_past = jnp.arange(n_blk)[None, :] < q_blk[:, None]
    gate = jnp.where(fully_past, gate, -jnp.inf)
    k_sel = min(MOBA_TOPK, n_blk)
    _, sel = lax.top_k(gate, k_sel)

    n_chunk = s_pad // MOBA_QCHUNK
    q_c = qf.reshape(b, h, n_chunk, MOBA_QCHUNK, d).transpose(2, 0, 1, 3, 4)
    sel_c = sel.reshape(b, h, n_chunk, MOBA_QCHUNK, k_sel).transpose(2, 0, 1, 3, 4)
    starts = jnp.arange(n_chunk, dtype=jnp.int32) * MOBA_QCHUNK
    b_ix = jnp.arange(b)[:, None, None, None]
    h_ix = jnp.arange(h)[None, :, None, None]
    offs = jnp.arange(MOBA_BLOCK)
    n_g = k_sel * MOBA_BLOCK

    def chunk(args):
        qc, sc, t0 = args
        q_pos = t0 + jnp.arange(MOBA_QCHUNK)
        blk = t0 // MOBA_BLOCK
        k_g = kb[b_ix, h_ix, sc]
        v_g = vb[b_ix, h_ix, sc]
        k_pos = sc[..., None] * MOBA_BLOCK + offs
        s_g = jnp.einsum("bhqd,bhqnkd->bhqnk", qc, k_g) * scale
        s_g = s_g - slopes[:, None, None, None] * (q_pos[:, None, None] - k_pos).astype(jnp.float32)
        s_g = jnp.where((jnp.arange(k_sel) < blk)[:, None], s_g, -jnp.inf)
        k_own = lax.dynamic_index_in_dim(kb, blk, axis=2, keepdims=False)
        v_own = lax.dynamic_index_in_dim(vb, blk, axis=2, keepdims=False)
        own_pos = blk * MOBA_BLOCK + offs
        s_o = jnp.einsum("bhqd,bhkd->bhqk", qc, k_own) * scale
        s_o = s_o - slopes[:, None, None] * (q_pos[:, None] - own_pos[None, :]).astype(jnp.float32)
        s_o = jnp.where(own_pos[None, :] <= q_pos[:, None], s_o, -jnp.inf)
        probs = jax.nn.softmax(
            jnp.concatenate([s_g.reshape(b, h, MOBA_QCHUNK, n_g), s_o], axis=-1), axis=-1)
        out = jnp.einsum("bhqn,bhqnd->bhqd", probs[..., :n_g],
                         v_g.reshape(b, h, MOBA_QCHUNK, n_g, d))
        out = out + jnp.einsum("bhqk,bhkd->bhqd", probs[..., n_g:], v_own)
        return out

    out = lax.map(chunk, (q_c, sel_c, starts))
    out = out.transpose(1, 2, 0, 3, 4).reshape(b, h, s_pad, d)[:, :, :s]
    return out.astype(v.dtype)


def setup_inputs(seed: int = 0) -> dict:
    key = jax.random.key(seed)
    ks = jax.random.split(key, 11)
    f32 = jnp.float32
    x = jax.random.normal(ks[0], (BATCH, SEQ, D_MODEL), f32)
    p = jax.random.normal(ks[1], (DEPTH, BATCH, SEQ, D_PLE), f32)
    w_in = jax.random.normal(ks[2], (DEPTH, D_MODEL, 4 * D_MIX), f32) * D_MODEL ** -0.5
    g_mix = 1.0 + 0.02 * jax.random.normal(ks[3], (DEPTH, D_MODEL), f32)
    g_out_sb = 1.0 + 0.02 * jax.random.normal(ks[4], (DEPTH, D_SB), f32)
    g_out_mb = 1.0 + 0.02 * jax.random.normal(ks[5], (DEPTH, D_MB), f32)
    w_out = jax.random.normal(ks[6], (DEPTH, D_MIX, D_MODEL), f32) * D_MIX ** -0.5
    w_ple = jax.random.normal(ks[7], (DEPTH, D_PLE, D_MODEL), f32) * D_PLE ** -0.5
    g_ple = 1.0 + 0.02 * jax.random.normal(ks[8], (DEPTH, D_MODEL), f32)
    w_ple_gate = jax.random.normal(ks[9], (DEPTH, D_MODEL, D_MODEL), f32) * D_MODEL ** -0.5
    g_final = 1.0 + 0.02 * jax.random.normal(ks[10], (D_MODEL,), f32)
    return {"x": x, "p": p, "w_in": w_in, "g_mix": g_mix, "g_out_sb": g_out_sb,
            "g_out_mb": g_out_mb, "w_out": w_out, "w_ple": w_ple, "g_ple": g_ple,
            "w_ple_gate": w_ple_gate, "g_final": g_final}


def reference(x, p, w_in, g_mix, g_out_sb, g_out_mb, w_out, w_ple, g_ple, w_ple_gate, g_final):
    slopes = alibi_slopes(N_HEADS_MB)
    widths = [D_SB] * 4 + [D_MB] * 4
    split_at = [int(o) for o in np.cumsum(widths)[:-1]]
    for i in range(DEPTH):
        h = rmsnorm(x, g_mix[i])
        proj = h @ w_in[i]
        q_sb, k_sb, v_sb, z_sb, q_mb, k_mb, v_mb, z_mb = jnp.split(proj, split_at, axis=-1)
        o_sb = merge_heads(stick_breaking_attention(
            split_heads(q_sb, N_HEADS_SB), split_heads(k_sb, N_HEADS_SB),
            split_heads(v_sb, N_HEADS_SB)))
        o_mb = merge_heads(moba_attention(
            split_heads(q_mb, N_HEADS_MB), split_heads(k_mb, N_HEADS_MB),
            split_heads(v_mb, N_HEADS_MB), slopes))
        y_sb = rmsnorm(o_sb, g_out_sb[i]) * jax.nn.silu(z_sb)
        y_mb = rmsnorm(o_mb, g_out_mb[i]) * jax.nn.silu(z_mb)
        x = x + jnp.concatenate([y_sb, y_mb], axis=-1) @ w_out[i]
        ple_gate = jax.nn.sigmoid(rmsnorm(x, g_ple[i]) @ w_ple_gate[i])
        x = x + (p[i] @ w_ple[i]) * ple_gate
    return rmsnorm(x, g_final)
```

```python
from contextlib import ExitStack

import numpy as np
import ml_dtypes
import concourse.bass as bass
import concourse.mybir as mybir
from concourse.bass_utils import run_bass_kernel_spmd

F32 = mybir.dt.float32
BF16 = mybir.dt.bfloat16
AF = mybir.ActivationFunctionType
ALU = mybir.AluOpType
AX = mybir.AxisListType

D = 1024
DPLE = 256
EPS = 1e-6
NEG = -30000.0
ENGS = ("pe", "act", "dve", "pool", "sp")


class Buf:
    __slots__ = ("name", "w", "r")

    def __init__(self, name=""):
        self.name = name
        self.w = None
        self.r = {}


class Prog:
    def __init__(self, n_dma_sp=12, n_dma_pool=6):
        self.ops = {e: [] for e in ENGS}
        self.cnt = {e: 0 for e in ENGS}
        self.seen = {e: {} for e in ENGS}
        self.dma_pool = {"sp": [("dma", "sp", i) for i in range(n_dma_sp)],
                         "pool": [("dma", "pool", i) for i in range(n_dma_pool)]}
        self.dma_rr = {"sp": 0, "pool": 0}
        self.dma_cnt = {}
        for q in self.dma_pool.values():
            for k in q:
                self.dma_cnt[k] = 0

    def op(self, eng, fn, reads=(), writes=(), dma=False):
        deps = {}

        def add(ev, kind):
            if ev is None:
                return
            key, val = ev
            if key == eng and (eng == "pe" or kind == "war"):
                return
            if deps.get(key, 0) < val:
                deps[key] = val

        for b in reads:
            add(b.w, "raw")
        for b in writes:
            add(b.w, "waw")
            for key, val in b.r.items():
                add((key, val), "war")
        if dma:
            pool = self.dma_pool[eng]
            k = pool[self.dma_rr[eng] % len(pool)]
            self.dma_rr[eng] += 1
            prev = self.dma_cnt[k]
            if prev > 0 and deps.get(k, 0) < prev:
                deps[k] = prev
            self.dma_cnt[k] = prev + 16
            ev = (k, prev + 16)
        else:
            self.cnt[eng] += 1
            ev = (eng, self.cnt[eng])
        seen = self.seen[eng]
        waits = []
        for key, val in deps.items():
            if seen.get(key, 0) < val:
                waits.append((key, val))
                seen[key] = val
        self.ops[eng].append((fn, waits, ev, dma))
        for b in reads:
            if b.r.get(ev[0], 0) < ev[1]:
                b.r[ev[0]] = ev[1]
        for b in writes:
            b.w = ev
            b.r = {}
        return ev

    def alias(self, new_bufs, old_bufs):
        merged = {}
        for b in old_bufs:
            evs = list(b.r.items())
            if b.w is not None:
                evs.append(b.w)
            for key, val in evs:
                if merged.get(key, 0) < val:
                    merged[key] = val
        for b in new_bufs:
            for key, val in merged.items():
                if b.r.get(key, 0) < val:
                    b.r[key] = val

    def emit(self, nc):
        sem_keys = list(ENGS[:4]) + [k for q in self.dma_pool.values() for k in q]
        with ExitStack() as st:
            sems = {}
            for k in sem_keys:
                nm = k if isinstance(k, str) else "d_%s_%d" % (k[1], k[2])
                sems[k] = st.enter_context(nc.semaphore("s_" + nm))
            final_waits = [(k, v) for k, v in self.dma_cnt.items() if v > 0]
            block = st.enter_context(nc.Block())

            def replay(name, e):
                for fn, waits, ev, dma in self.ops[name]:
                    for key, val in waits[:-1]:
                        e.wait_ge(sems[key], val)
                    ins = fn(e)
                    if waits:
                        ins._wait_ge(sems[waits[-1][0]], waits[-1][1])
                    ins.then_inc(sems[ev[0]], 16 if dma else 1)
                if name == "sp":
                    for key, val in final_waits:
                        e.wait_ge(sems[key], val)

            @block.tensor
            def _(e):
                replay("pe", e)

            @block.scalar
            def _(e):
                replay("act", e)

            @block.vector
            def _(e):
                replay("dve", e)

            @block.gpsimd
            def _(e):
                replay("pool", e)

            @block.sync
            def _(e):
                replay("sp", e)


def _consts(S):
    bf = ml_dtypes.bfloat16
    NT = S // 128
    i = np.arange(128)
    c = {}
    c["ident"] = np.eye(128, dtype=np.float32).astype(bf)
    c["negtri"] = (-(i[:, None] >= i[None, :]).astype(np.float32)).astype(bf)
    c["negident"] = (-np.eye(128, dtype=np.float32)).astype(bf)
    c["ones"] = np.ones((128, 128), np.float32).astype(bf)
    c["negones"] = (-np.ones((128, 128), np.float32)).astype(bf)
    c["masksb"] = np.where(i[:, None] < i[None, :], 0.0, NEG).astype(np.float32).astype(bf)
    c["maskmb"] = np.where(i[:, None] <= i[None, :], 0.0, NEG).astype(np.float32).astype(bf)
    c["onesf"] = np.ones((128, 64), np.float32)
    pos = np.arange(S)
    hi = (pos // 256) * 256.0
    lo = (pos % 256) * 1.0
    kaug = np.zeros((8, 20, S), np.float32)
    qaug = np.zeros((8, 4, S), np.float32)
    for h in range(8):
        sl = 2.0 ** (-8.0 * (h + 1) / 8.0)
        for n in range(16):
            kaug[h, n] = (pos // 256 == n)
        kaug[h, 16] = 1.0
        kaug[h, 17] = 1.0
        kaug[h, 18] = sl * hi
        kaug[h, 19] = sl * lo
        qaug[h, 0] = -sl * hi
        qaug[h, 1] = -sl * lo
        qaug[h, 2] = 1.0
        qaug[h, 3] = 1.0
    c["kaug"] = kaug.astype(bf)
    c["qaug"] = qaug.astype(bf)
    gmask = np.zeros((NT, 16), np.float32)
    negcand = np.zeros((NT, 16), np.float32)
    fixed = np.zeros((NT, 16), np.float32)
    for ti in range(NT):
        qb = ti // 2
        for n in range(16):
            if n < qb:
                negcand[ti, n] = NEG
            else:
                gmask[ti, n] = -1e30
            if n > qb:
                fixed[ti, n] = NEG
    c["gmask"] = np.broadcast_to(gmask.reshape(1, NT * 16), (128, NT * 16)).astype(bf)
    c["negcand"] = np.broadcast_to(negcand.reshape(1, NT * 16), (128, NT * 16)).astype(bf)
    c["fixed"] = np.broadcast_to(fixed.reshape(1, NT * 16), (128, NT * 16)).astype(bf)
    return c


def build(S, dbg=None, stop_after=None):
    dbg = set(dbg or ())
    if "qk" in dbg:
        dbg |= {"T0", "T1", "T2", "T3", "vt"}
    NT = S // 128
    NQ = S // 512
    nc = bass.Bass("TRN2", target_bir_lowering=False)
    P = Prog()

    def din(name, shape, dt=F32):
        return nc.dram_tensor(name, list(shape), dt, kind="ExternalInput").ap()

    x_d = din("x", [S, D])
    p_d = din("p", [S, DPLE])
    w_in_d = din("w_in", [D, 4 * D])
    w_out_d = din("w_out", [D, D])
    w_ple_d = din("w_ple", [DPLE, D])
    w_gate_d = din("w_gate", [D, D])
    gmix_d = din("gmix_bc", [128, D])
    gfin_d = din("gfin_bc", [128, D])
    gout_d = din("gout_pc", [128, 8])
    gple_d = din("gple_pc", [128, 8])
    ident_d = din("ident", [128, 128], BF16)
    negtri_d = din("negtri", [128, 128], BF16)
    ones_d = din("ones", [128, 128], BF16)
    negones_d = din("negones", [128, 128], BF16)
    negident_d = din("negident", [128, 128], BF16)
    masksb_d = din("masksb", [128, 128], BF16)
    maskmb_d = din("maskmb", [128, 128], BF16)
    onesf_d = din("onesf", [128, 64])
    kaug_d = din("kaug", [8, 20, S], BF16)
    qaug_d = din("qaug", [8, 4, S], BF16)
    gmask_d = din("gmask", [128, NT * 16], BF16)
    negcand_d = din("negcand", [128, NT * 16], BF16)
    fixed_d = din("fixed", [128, NT * 16], BF16)
    out_d = nc.dram_tensor("out", [S, D], F32, kind="ExternalOutput").ap()

    dumps = {}
    top = ExitStack()

    def sb(name, shape, dt, st=top):
        return st.enter_context(nc.sbuf_tensor("sb_" + name, list(shape), dt))

    def dump(name, src_ap, shape, dt, reads):
        if name not in dbg:
            return
        t = nc.dram_tensor("dbg_" + name, list(shape), dt, kind="ExternalOutput").ap()
        dumps[name] = t
        P.op("sp", lambda e, t=t, s=src_ap: e.dma_start(out=t, in_=s), reads=reads, dma=True)

    def mm(out, lhsT, rhs, start, reads, writes, stop=True):
        P.op("pe", lambda e: e.matmul(out, lhsT, rhs, start=start, stop=stop,
                                      skip_group_check=True), reads=reads, writes=writes)

    def tr(out, in_, reads, writes):
        P.op("pe", lambda e: e.transpose(out=out, in_=in_, identity=ident[:]),
             reads=list(reads) + [cbuf], writes=writes)

    def act(out, in_, func, reads, writes, **kw):
        P.op("act", lambda e: e.activation(out=out, in_=in_, func=func, **kw),
             reads=reads, writes=writes)

    def dma(eng, out, in_, reads, writes):
        P.op(eng, lambda e: e.dma_start(out=out, in_=in_), reads=reads, writes=writes, dma=True)

    def vop(eng, name, reads, writes, **kw):
        P.op(eng, lambda e: getattr(e, name)(**kw), reads=reads, writes=writes)

    def rstd_op(dst, src, scale, b):
        act(dst, src, AF.Sqrt, [b, cbuf], [b], scale=scale, bias=epsc[0:dst.shape[0], 0:1])
        vop("dve", "reciprocal", [b], [b], out=dst, in_=dst)

    def copy_any(k, out, in_, reads, writes):
        if k % 2 == 0:
            P.op("act", lambda e: e.copy(out=out, in_=in_), reads=reads, writes=writes)
        else:
            P.op("dve", lambda e: e.tensor_copy(out=out, in_=in_), reads=reads, writes=writes)

    ident = sb("ident", [128, 128], BF16)
    negtri = sb("negtri", [128, 128], BF16)
    ones = sb("ones", [128, 128], BF16)
    negones = sb("negones", [128, 128], BF16)
    negident = sb("negident", [128, 128], BF16)
    masksb = sb("masksb", [128, 128], BF16)
    maskmb = sb("maskmb", [128, 128], BF16)
    onesf = sb("onesf", [128, 64], F32)
    gout = sb("gout", [128, 8], F32)
    gple = sb("gple", [128, 8], F32)
    epsc = sb("epsc", [128, 1], F32)
    cbuf = Buf("consts")
    P.op("dve", lambda e: e.memset(epsc[:], EPS), writes=[cbuf])
    for t, d in ((ident, ident_d), (negtri, negtri_d), (ones, ones_d), (negones, negones_d),
                 (negident, negident_d), (masksb, masksb_d), (maskmb, maskmb_d), (onesf, onesf_d), (gout, gout_d),
                 (gple, gple_d)):
        dma("sp", t[:], d, [], [cbuf])

    gz = sb("gz", [128, 8, S], BF16)
    gz_b = [[Buf("gz%d_%d" % (pr, qt)) for qt in range(NQ)] for pr in range(8)]
    ssq = sb("ssq", [128, 8, NT], F32)
    ssq_b = Buf("ssq")

    banks = [top.enter_context(nc.psum_tensor("bank%d" % i, [128, 512], F32)) for i in range(8)]
    bank_b = [Buf("bank%d" % i) for i in range(8)]

    def bank_bf(i):
        return banks[i][:].bitcast(BF16)

    stB = ExitStack()
    hT = sb("hT", [128, 8, S], BF16, stB)
    hT_b = [Buf("hT%d" % i) for i in range(NT)]

    stA = ExitStack()
    gmix = sb("gmix", [128, D], F32, stA)
    gmix_b = Buf("gmix")
    dma("sp", gmix[:], gmix_d, [], [gmix_b])
    xt = [sb("xt%d" % i, [128, D], F32, stA) for i in range(2)]
    xt_b = [Buf("xt%d" % i) for i in range(2)]
    junk = sb("junkA", [128, D], BF16, stA)
    junk_b = Buf("junkA")
    hb = [sb("hb%d" % i, [128, D], BF16, stA) for i in range(2)]
    hb_b = [Buf("hb%d" % i) for i in range(2)]
    st1 = [sb("st1_%d" % i, [128, 2], F32, stA) for i in range(2)]
    st1_b = [Buf("st1_%d" % i) for i in range(2)]
    phaseA_bufs = [gmix_b, junk_b] + xt_b + hb_b + st1_b

    def A_norm(i):
        s = i % 2
        dma("sp", xt[s][:], x_d[i * 128:(i + 1) * 128, :], [], [xt_b[s]])
        act(junk[:], xt[s][:], AF.Square, [xt_b[s]], [junk_b, st1_b[s]], accum_out=st1[s][:, 0:1])
        rstd_op(st1[s][:, 1:2], st1[s][:, 0:1], 1.0 / D, st1_b[s])
        vop("dve", "scalar_tensor_tensor", [xt_b[s], st1_b[s], gmix_b], [hb_b[s]],
            out=hb[s][:], in0=xt[s][:], scalar=st1[s][:, 1:2], in1=gmix[:],
            op0=ALU.mult, op1=ALU.mult)

    def A_tr(i):
        s = i % 2
        bk = 6 + (i % 2)
        pT = bank_bf(bk)
        for c in range(8):
            tr(pT[:, c * 128:(c + 1) * 128], hb[s][:, c * 128:(c + 1) * 128], [hb_b[s]], [bank_b[bk]])
        copy_any(i, hT[:, :, i * 128:(i + 1) * 128], pT.rearrange("p (c t) -> p c t", c=8),
                 [bank_b[bk]], [hT_b[i]])

    for i in range(NT + 1):
        if i < NT:
            A_norm(i)
        if i >= 1:
            A_tr(i - 1)
    dump("hT", hT[:], [128, 8, S], BF16, hT_b)
    stA.close()
    if stop_after == "A":
        P.emit(nc)
        return nc, dumps

    T = [sb("T%d" % i, [128, S], BF16, stB) for i in range(4)]
    T_b = [Buf("T%d" % i) for i in range(4)]
    P.alias(T_b, phaseA_bufs)
    vt = sb("vt", [128, NT, 130], BF16, stB)
    vt_b = Buf("vt")
    wbuf = [sb("wbuf0", [128, 8, 4, 128], BF16, stB)] * 2
    wbuf_b = [Buf("wbuf0")] * 2
    scr = sb("scr", [128, 3584], F32, stB)
    esb = [scr[:, 0:512], scr[:, 512:1024]]
    esb_b = [Buf("esb%d" % i) for i in range(2)]
    spb = [scr[:, 1024:1280].bitcast(BF16), scr[:, 1280:1536].bitcast(BF16)]
    spb_b = [Buf("spb%d" % i) for i in range(2)]
    rrow = [scr[:, 1536:1792].bitcast(BF16), scr[:, 1792:2048].bitcast(BF16)]
    rrow_b = [Buf("rrow%d" % i) for i in range(2)]
    sb_only = esb_b + spb_b + rrow_b
    rden = [scr[:, 0:512], scr[:, 2560:3072], scr[:, 3072:3584]]
    rden_b = [Buf("rden%d" % i) for i in range(3)]
    bcsb = scr[0:64, 512:1024]
    bcsb_b = Buf("bcsb")
    gsb = scr[:, 1024:1024 + NT * 16].rearrange("p (t n) -> p t n", n=16)
    gsb_b = Buf("gsb")
    m8 = scr[:, 1536:1536 + NT * 8].rearrange("p (t n) -> p t n", n=8)
    m8_b = Buf("m8")
    biasq = scr[:, 1792:2048].bitcast(BF16)[:, 0:NT * 16].rearrange("p (t n) -> p t n", n=16)
    biasq_b = Buf("biasq")
    biasT = scr[0:16, 2048:2560].bitcast(BF16)
    biasT_b = Buf("biasT")
    mb_only = rden_b + [bcsb_b, gsb_b, m8_b, biasq_b, biasT_b]
    ptb = [sb("ptb%d" % i, [128, 512], BF16, stB) for i in range(2)]
    ptb_b = [Buf("ptb%d" % i) for i in range(2)]
    opair = [sb("opair%d" % i, [128, 512], F32, stB) for i in range(2)]
    opair_b = [Buf("opair%d" % i) for i in range(2)]
    ostage = [sb("ostage0", [64, 512], F32, stB)] * 2
    ostage_b = [Buf("ostage0")] * 2
    osq = sb("osq", [128, 512], BF16, stB)
    osq_b = Buf("osq")
    gmask = sb("gmask", [128, NT * 16], BF16, stB)
    negcand = sb("negcand", [128, NT * 16], BF16, stB)
    fixed = sb("fixed", [128, NT * 16], BF16, stB)
    gconst_b = Buf("gconst")
    ksf = sb("ksf", [64, 16], F32, stB)
    ksb = sb("ksb", [64, 16], BF16, stB)
    ks_b = Buf("ks")
    P.alias([vt_b, gconst_b, ks_b, osq_b, wbuf_b[0], opair_b[0], opair_b[1], ostage_b[0]]
            + ptb_b + sb_only, phaseA_bufs)
    for t, d in ((gmask, gmask_d), (negcand, negcand_d), (fixed, fixed_d)):
        dma("sp", t[:], d, [], [gconst_b])
    vop("pool", "memset", [], [vt_b], ap=vt[:], constant=1.0)
    vop("dve", "memset", [], [ks_b], ap=ksb[:], constant=0.0)

    w_in_v = w_in_d.rearrange("(c p) n -> p c n", p=128)

    def load_w(pr):
        G, j = pr // 4, pr % 4
        wb = wbuf[pr % 2]
        for t in range(4):
            col0 = G * 2048 + t * 512 + j * 128
            dma("pool", wb[:, :, t, :], w_in_v[:, :, col0:col0 + 128], [], [wbuf_b[pr % 2]])

    def in_proj(pr):
        G = pr // 4
        wb = wbuf[pr % 2]
        wb_b = wbuf_b[pr % 2]
        rot = [6, 7, 0, 1, 2]
        k = [0]

        def nb():
            k[0] += 1
            return rot[k[0] % len(rot)]

        for t, scale in ((0, 0.125), (1, None)):
            for tq in range(NQ):
                cols = slice(tq * 512, (tq + 1) * 512)
                ti_ = t if G == 0 else 2 * t
                groups = [(slice(0, 128), T[ti_], T_b[ti_], 128)]
                for wc, dst, dst_b, M in groups:
                    b = nb()
                    for c in range(8):
                        mm(banks[b][0:M, :], wb[:, c, t, wc], hT[:, c, cols], c == 0,
                           [wb_b] + hT_b[tq * 4:(tq + 1) * 4], [bank_b[b]], stop=(c == 7))
                    if scale is not None:
                        if k[0] % 2 == 0:
                            act(dst[0:M, cols], banks[b][0:M, :], AF.Copy, [bank_b[b]], [dst_b],
                                scale=scale)
                        else:
                            vop("dve", "tensor_scalar", [bank_b[b]], [dst_b], out=dst[0:M, cols],
                                in0=banks[b][0:M, :], scalar1=scale, scalar2=None, op0=ALU.mult)
                    else:
                        copy_any(k[0], dst[0:M, cols], banks[b][0:M, :], [bank_b[b]], [dst_b])
        if G == 1:
            for t in range(2):
                dma("sp", T[2 * t + 1][0:64, :], T[2 * t][64:128, :], [T_b[2 * t]], [T_b[2 * t + 1]])
        for tq in range(NQ):
            cols = slice(tq * 512, (tq + 1) * 512)
            b = nb()
            for c in range(8):
                mm(banks[b][:, :], wb[:, c, 3, :], hT[:, c, cols], c == 0,
                   [wb_b] + hT_b[tq * 4:(tq + 1) * 4], [bank_b[b]], stop=(c == 7))
            act(gz[:, pr, cols], banks[b][:, :], AF.Silu, [bank_b[b]], [gz_b[pr][tq]])
        for g in range(NT // 4):
            b = nb()
            for tt in range(4):
                ti = g * 4 + tt
                for c in range(8):
                    mm(banks[b][:, tt * 128:(tt + 1) * 128], hT[:, c, ti * 128:(ti + 1) * 128],
                       wb[:, c, 2, :], c == 0, [wb_b, hT_b[ti]], [bank_b[b]], stop=(c == 7))
            bv = banks[b][:].rearrange("p (t n) -> p t n", t=4)
            P.op("act", lambda e, g=g, bv=bv: e.copy(out=vt[:, g * 4:(g + 1) * 4, 0:64],
                                                     in_=bv[:, :, 0:64]),
                 reads=[bank_b[b]], writes=[vt_b])
            P.op("dve", lambda e, g=g, bv=bv: e.tensor_copy(out=vt[:, g * 4:(g + 1) * 4, 65:129],
                                                            in_=bv[:, :, 64:128]),
                 reads=[bank_b[b]], writes=[vt_b])

    def moba_gate(h, qT, qT_b, kT, kT_b):
        dma("sp", kT[64:84, :], kaug_d[h], [], [kT_b])
        dma("sp", qT[80:84, :], qaug_d[h], [], [qT_b])
        NB = S // 256
        vop("dve", "tensor_reduce", [kT_b], [ks_b], out=ksf[:, 0:NB],
            in_=kT[0:64, :].rearrange("p (n s) -> p n s", s=256), axis=AX.X, op=ALU.add)
        vop("dve", "tensor_copy", [ks_b], [ks_b], out=ksb[:, 0:NB], in_=ksf[:, 0:NB])
        gb = 6
        for ti in range(NT):
            mm(banks[gb][:, ti * 16:(ti + 1) * 16], qT[0:64, ti * 128:(ti + 1) * 128], ksb[:, :],
               True, [qT_b, ks_b], [bank_b[gb]])
        vop("dve", "tensor_tensor", [bank_b[gb], gconst_b], [gsb_b],
            out=gsb, in0=banks[gb][:, 0:NT * 16].rearrange("p (t n) -> p t n", n=16),
            in1=gmask[:].rearrange("p (t n) -> p t n", n=16), op=ALU.add)
        for ti in range(NT):
            vop("dve", "max", [gsb_b], [m8_b], out=m8[:, ti, :], in_=gsb[:, ti, :])
        vop("dve", "tensor_tensor", [gsb_b, m8_b], [gsb_b], out=gsb, in0=gsb,
            in1=m8[:, :, 2:3].to_broadcast([128, NT, 16]), op=ALU.is_lt)
        vop("dve", "tensor_tensor", [gsb_b, gconst_b], [gsb_b], out=gsb, in0=gsb,
            in1=negcand[:].rearrange("p (t n) -> p t n", n=16), op=ALU.mult)
        vop("dve", "tensor_tensor", [gsb_b, gconst_b], [biasq_b], out=biasq, in0=gsb,
            in1=fixed[:].rearrange("p (t n) -> p t n", n=16), op=ALU.add)
        tb = 7
        pT = bank_bf(tb)
        for g in range((NT + 7) // 8):
            n = min(8, NT - g * 8)
            for tt in range(n):
                ti = g * 8 + tt
                tr(pT[0:16, tt * 128:(tt + 1) * 128], biasq[:, ti, :], [biasq_b], [bank_b[tb]])
            copy_any(g, biasT[:, 0:n * 128], pT[0:16, 0:n * 128], [bank_b[tb]], [biasT_b])
            dma("sp", qT[64:80, g * 1024:g * 1024 + n * 128], biasT[:, 0:n * 128], [biasT_b], [qT_b])

    ZB = [0, 1, 2]
    OB = [3, 4, 7]
    RB = 5
    MB = 6

    def attention(pr):
        G = pr // 4
        heads = []
        for hh in range(2):
            if G == 0:
                heads.append(dict(q=T[0], q_b=T_b[0], k=T[1], k_b=T_b[1], pb=64 * hh, K=64))
            else:
                heads.append(dict(q=T[hh], q_b=T_b[hh], k=T[2 + hh], k_b=T_b[2 + hh], pb=0, K=84))
        mask = masksb if G == 0 else maskmb
        steps = []
        gcount = 0
        for qt in (range(NQ) if G == 0 else range(NQ - 1, -1, -1)):
            for hh in range(2):
                kts = list(range(4 * qt + 3, -1, -1))
                for si, kt in enumerate(kts):
                    steps.append(dict(qt=qt, hh=hh, kt=kt, si=si, last=(si == len(kts) - 1),
                                      idx=len(steps), g=gcount))
                gcount += 1
        n = len(steps)

        def geom(st):
            j = st["kt"] - 4 * st["qt"]
            c0 = 128 * j if j > 0 else 0
            return j, c0

        def emit_Z(st):
            hd = heads[st["hh"]]
            j, c0 = geom(st)
            zb = ZB[st["idx"] % 3]
            pb, K = hd["pb"], hd["K"]
            kt, qt = st["kt"], st["qt"]
            mm(banks[zb][:, c0:512], hd["k"][pb:pb + K, kt * 128:(kt + 1) * 128],
               hd["q"][pb:pb + K, qt * 512 + c0:(qt + 1) * 512], True,
               [hd["k_b"], hd["q_b"]], [bank_b[zb]], stop=False)
            if j >= 0:
                mm(banks[zb][:, c0:c0 + 128], ident[:], mask[:], False, [cbuf], [bank_b[zb]],
                   stop=False)

        def emit_A1(st):
            j, c0 = geom(st)
            zb = ZB[st["idx"] % 3]
            s2 = st["idx"] % 2
            act(esb[s2][:, c0:512], banks[zb][:, c0:512], AF.Exp, [bank_b[zb]], [esb_b[s2]])

        def emit_A2(st):
            j, c0 = geom(st)
            s2 = st["idx"] % 2
            act(spb[s2][:, c0:512], esb[s2][:, c0:512], AF.Ln, [esb_b[s2]], [spb_b[s2]], bias=1.0)

        def emit_tri(st):
            j, c0 = geom(st)
            zb = ZB[st["idx"] % 3]
            s2 = st["idx"] % 2
            mm(banks[zb][:, c0:512], negtri[:], spb[s2][:, c0:512], False,
               [cbuf, spb_b[s2]], [bank_b[zb]], stop=False)
            if st["si"] > 0:
                cR = geom(steps[st["idx"] - 1])[1]
                mm(banks[zb][:, cR:512], negident[:], rrow[s2][:, cR:512], False,
                   [cbuf, rrow_b[s2]], [bank_b[zb]], stop=True)
            if not st["last"]:
                mm(banks[RB][:, c0:512], ones[:, :], spb[s2][:, c0:512], st["si"] == 0,
                   [cbuf, spb_b[s2]], [bank_b[RB]], stop=False)
                n2 = (st["idx"] + 1) % 2
                vop("dve", "tensor_copy", [bank_b[RB]], [rrow_b[n2]], out=rrow[n2][:, c0:512],
                    in_=banks[RB][:, c0:512])

        def emit_A3(st):
            j, c0 = geom(st)
            zb = ZB[st["idx"] % 3]
            s2 = st["idx"] % 2
            act(ptb[s2][:, c0:512], banks[zb][:, c0:512], AF.Exp, [bank_b[zb]], [ptb_b[s2]])

        def emit_PV(st):
            j, c0 = geom(st)
            s2 = st["idx"] % 2
            hh, qt, kt = st["hh"], st["qt"], st["kt"]
            ob = OB[st["g"] % 3]
            M = 64 if G == 0 else 65
            mm(banks[ob][0:M, c0:512], vt[:, kt, hh * 65:hh * 65 + M], ptb[s2][:, c0:512],
               st["si"] == 0, [vt_b, ptb_b[s2]], [bank_b[ob]], stop=st["last"])
            if st["last"]:
                if G == 0:
                    pending.append([3, "fin", st, ob])
                else:
                    pending.append([1, "ln", st, ob])

        def emit_evac(st, ob):
            hh, qt = st["hh"], st["qt"]
            op_i = qt % 2
            cols = slice(qt * 512, (qt + 1) * 512)
            if hh == 0:
                dst, dst_b = opair[op_i][0:64, :], opair_b[op_i]
            else:
                dst, dst_b = ostage[qt % 2][:, :], ostage_b[qt % 2]
            if G == 0:
                copy_any(qt + hh, dst, banks[ob][0:64, :], [bank_b[ob]], [dst_b])
            else:
                act(bcsb, banks[MB][0:64, :], AF.Exp, [bank_b[MB]], [bcsb_b], scale=-1.0)
                vop("dve", "tensor_tensor", [bank_b[ob], bcsb_b], [dst_b], out=dst,
                    in0=banks[ob][0:64, :], in1=bcsb, op=ALU.mult)
            if hh == 1:
                dma("sp", opair[op_i][64:128, :], ostage[qt % 2][:, :], [ostage_b[qt % 2]],
                    [opair_b[op_i]])
                vop("pool", "tensor_tensor", [opair_b[op_i]], [osq_b], out=osq[:, :],
                    in0=opair[op_i][:, :], in1=opair[op_i][:, :], op=ALU.mult)
                for tt in range(4):
                    mm(banks[MB][:, 508 + tt:509 + tt], osq[:, tt * 128:(tt + 1) * 128],
                       ones[:, 0:1], True, [osq_b, cbuf], [bank_b[MB]])
                vop("dve", "tensor_copy", [bank_b[MB]], [ssq_b], out=ssq[:, pr, qt * 4:(qt + 1) * 4],
                    in_=banks[MB][:, 508:512])
                vop("dve", "tensor_tensor", [opair_b[op_i], gz_b[pr][qt]], [gz_b[pr][qt]],
                    out=gz[:, pr, cols], in0=opair[op_i][:, :], in1=gz[:, pr, cols], op=ALU.mult)

        pending = []

        def run_stage(kind, st, ob):
            ri = st["g"] % 3
            if kind == "ln":
                act(rden[ri][64:65, :], banks[ob][64:65, :], AF.Ln, [bank_b[ob]], [rden_b[ri]])
                pending.append([2, "bc", st, ob])
            elif kind == "bc":
                mm(banks[MB][0:64, :], onesf[64:65, 0:64], rden[ri][64:65, :], True,
                   [cbuf, rden_b[ri]], [bank_b[MB]])
                pending.append([2, "fin", st, ob])
            else:
                emit_evac(st, ob)

        def flush(all_=False):
            while True:
                for pe_ in list(pending):
                    pe_[0] -= 1
                    if pe_[0] <= 0 or all_:
                        pending.remove(pe_)
                        run_stage(pe_[1], pe_[2], pe_[3])
                if not (all_ and pending):
                    break

        if G == 0:
            emit_Z(steps[0])
            for i in range(n + 2):
                if 0 <= i - 1 < n:
                    emit_tri(steps[i - 1])
                if 0 <= i - 2 < n:
                    emit_PV(steps[i - 2])
                if i + 1 < n:
                    emit_Z(steps[i + 1])
                if i < n:
                    emit_A1(steps[i])
                if 0 <= i - 1 < n:
                    emit_A3(steps[i - 1])
                if i < n:
                    emit_A2(steps[i])
                flush()
        else:
            emit_Z(steps[0])
            if n > 1:
                emit_Z(steps[1])
            for i in range(n + 1):
                if i < n:
                    emit_A3(steps[i])
                if 0 <= i - 1 < n:
                    emit_PV(steps[i - 1])
                if i + 2 < n:
                    emit_Z(steps[i + 2])
                flush()
        flush(True)

    npairs = 8 if stop_after is None else int(stop_after[1:]) if stop_after.startswith("B") else 8
    load_w(0)
    for pr in range(npairs):
        if pr == 4:
            P.alias(mb_only, sb_only)
        in_proj(pr)
        if pr + 1 < 8:
            load_w(pr + 1)
        if pr // 4 == 1:
            for hh in range(2):
                moba_gate((pr % 4) * 2 + hh, T[hh], T_b[hh], T[2 + hh], T_b[2 + hh])
        if "qk" in dbg and pr == int(next(iter(d for d in dbg if d.startswith("pair="))).split("=")[1]):
            for i in range(2 if pr < 4 else 4):
                dump("T%d" % i, T[i][:] if pr < 4 else T[i][0:84, :], [128 if pr < 4 else 84, S], BF16,
                     [T_b[i]])
            dump("vt", vt[:], [128, NT, 130], BF16, [vt_b])
        attention(pr)
    dump("gz", gz[:, 0:npairs, :], [128, npairs, S], BF16, [b for l in gz_b for b in l])
    dump("ssq", ssq[:, 0:npairs, :], [128, npairs, NT], F32, [ssq_b])
    phaseB_bufs = (hT_b + T_b + [vt_b, gconst_b, ks_b, osq_b, wbuf_b[0], ostage_b[0]] + ptb_b + opair_b
                   + sb_only + mb_only)
    stB.close()
    if stop_after is not None and stop_after.startswith("B"):
        P.emit(nc)
        return nc, dumps

    stC = ExitStack()
    wo = sb("wo", [128, 8, D], BF16, stC)
    wg = sb("wg", [128, 8, D], BF16, stC)
    wp = sb("wp", [128, 2, D], BF16, stC)
    wo_b, wg_b, wp_b = Buf("wo"), Buf("wg"), Buf("wp")
    wst = [sb("wst%d" % i, [128, D], F32, stC) for i in range(2)]
    wst_b = [Buf("wst%d" % i) for i in range(2)]
    gfin = sb("gfin", [128, D], F32, stC)
    gfin_b = Buf("gfin")

    def mk(name, n, shape, dt):
        return ([sb("%s%d" % (name, i), shape, dt, stC) for i in range(n)],
                [Buf("%s%d" % (name, i)) for i in range(n)])

    xc, xc_b = mk("xc", 2, [128, D], F32)
    pc, pc_b = mk("pc", 2, [128, DPLE], F32)
    pcb, pcb_b = mk("pcb", 2, [128, DPLE], BF16)
    pT_sb, pT_b = mk("pTsb", 2, [128, 2, 128], BF16)
    x1, x1_b = mk("x1_", 3, [128, D], F32)
    x1b, x1b_b = mk("x1b_", 2, [128, D], BF16)
    x1T, x1T_b = mk("x1T_", 2, [128, 8, 128], BF16)
    gate, gate_b = mk("gate", 2, [128, D], F32)
    yo, yo_b = mk("yo", 2, [128, D], F32)
    stt, stt_b = mk("stt", 3, [128, 8], F32)
    junkc = sb("junkc", [128, D], BF16, stC)
    junkc_b = Buf("junkc")
    newC = ([wo_b, wg_b, wp_b, gfin_b, junkc_b] + wst_b + xc_b + pc_b + pcb_b + pT_b + x1_b + x1b_b
            + x1T_b + gate_b + yo_b + stt_b)
    P.alias(newC, phaseB_bufs)

    dma("sp", gfin[:], gfin_d, [], [gfin_b])
    w_out_v = w_out_d.rearrange("(c p) n -> p c n", p=128)
    w_gate_v = w_gate_d.rearrange("(c p) n -> p c n", p=128)
    k = 0
    for (wv, wt, wt_b, gv) in ((w_out_v, wo, wo_b, gout), (w_gate_v, wg, wg_b, gple)):
        for c in range(8):
            s = k % 2
            dma("sp", wst[s][:], wv[:, c, :], [], [wst_b[s]])
            eng = "dve" if k % 2 == 0 else "act"
            if eng == "dve":
                vop("dve", "tensor_scalar", [wst_b[s], cbuf], [wt_b], out=wt[:, c, :], in0=wst[s][:],
                    scalar1=gv[:, c:c + 1], scalar2=None, op0=ALU.mult)
            else:
                act(wt[:, c, :], wst[s][:], AF.Copy, [wst_b[s], cbuf], [wt_b], scale=gv[:, c:c + 1])
            k += 1
    dma("pool", wp[:], w_ple_d.rearrange("(c p) n -> p c n", p=128), [], [wp_b])

    rg = sb("rg", [128, NT, 2], F32, stC)
    rg_b = Buf("rg")
    P.alias([rg_b], phaseB_bufs)
    vop("dve", "tensor_reduce", [ssq_b], [rg_b], out=rg[:],
        in_=ssq[:].rearrange("p (g c) t -> p t g c", g=2), axis=AX.X, op=ALU.add)
    rstd_op(rg[:].rearrange("p t g -> p (t g)"), rg[:].rearrange("p t g -> p (t g)"), 1.0 / 512, rg_b)

    def OPX(i):
        s2, s3 = i % 2, i % 3
        tok = slice(i * 128, (i + 1) * 128)
        dma("sp", xc[s2][:], x_d[tok, :], [], [xc_b[s2]])
        dma("sp", pc[s2][:], p_d[tok, :], [], [pc_b[s2]])
        for g in range(2):
            for hf in range(2):
                b = g * 2 + hf
                for c4 in range(4):
                    c = g * 4 + c4
                    mm(banks[b][:, :], gz[:, c, tok], wo[:, c, hf * 512:(hf + 1) * 512], c4 == 0,
                       gz_b[c] + [wo_b], [bank_b[b]], stop=(c4 == 3))
        for hf in range(2):
            cs = slice(hf * 512, (hf + 1) * 512)
            vop("dve", "scalar_tensor_tensor", [bank_b[hf], rg_b, xc_b[s2]], [x1_b[s3]],
                out=x1[s3][:, cs], in0=banks[hf][:, :], scalar=rg[:, i, 0:1], in1=xc[s2][:, cs],
                op0=ALU.mult, op1=ALU.add)
            vop("dve", "scalar_tensor_tensor", [bank_b[2 + hf], rg_b, x1_b[s3]], [x1_b[s3]],
                out=x1[s3][:, cs], in0=banks[2 + hf][:, :], scalar=rg[:, i, 1:2], in1=x1[s3][:, cs],
                op0=ALU.mult, op1=ALU.add)
        vop("pool", "tensor_copy", [pc_b[s2]], [pcb_b[s2]], out=pcb[s2][:], in_=pc[s2][:])
        pT5 = bank_bf(5)
        for c in range(2):
            tr(pT5[:, c * 128:(c + 1) * 128], pcb[s2][:, c * 128:(c + 1) * 128], [pcb_b[s2]], [bank_b[5]])
        copy_any(1, pT_sb[s2][:], pT5[:, 0:256].rearrange("p (c t) -> p c t", c=2), [bank_b[5]],
                 [pT_b[s2]])

    def CST(i):
        s2, s3 = i % 2, i % 3
        P.op("act", lambda e: e.copy(out=x1b[s2][:], in_=x1[s3][:]), reads=[x1_b[s3]],
             writes=[x1b_b[s2]])
        act(junkc[:], x1[s3][:], AF.Square, [x1_b[s3]], [junkc_b, stt_b[s3]],
            accum_out=stt[s3][:, 2:3])
        rstd_op(stt[s3][:, 3:4], stt[s3][:, 2:3], 1.0 / D, stt_b[s3])

    def TRE(i):
        s2, s3 = i % 2, i % 3
        pT = bank_bf(4)
        for c in range(8):
            tr(pT[:, c * 128:(c + 1) * 128], x1b[s2][:, c * 128:(c + 1) * 128], [x1b_b[s2]], [bank_b[4]])
        copy_any(0, x1T[s2][:], pT.rearrange("p (c t) -> p c t", c=8), [bank_b[4]], [x1T_b[s2]])

    def S2(i):
        s2, s3 = i % 2, i % 3
        for hf in range(2):
            cs = slice(hf * 512, (hf + 1) * 512)
            gb = 6 if hf == 0 else 5
            for c in range(8):
                mm(banks[gb][:, :], x1T[s2][:, c, :], wg[:, c, cs], c == 0, [x1T_b[s2], wg_b],
                   [bank_b[gb]], stop=(c == 7))
            act(gate[s2][:, cs], banks[gb][:, :], AF.Sigmoid, [bank_b[gb], stt_b[s3]], [gate_b[s2]],
                scale=stt[s3][:, 3:4])
            for c in range(2):
                mm(banks[7][:, :], pT_sb[s2][:, c, :], wp[:, c, cs], c == 0, [pT_b[s2], wp_b],
                   [bank_b[7]], stop=(c == 1))
            vop("dve", "tensor_tensor", [bank_b[7], gate_b[s2]], [gate_b[s2]], out=gate[s2][:, cs],
                in0=banks[7][:, :], in1=gate[s2][:, cs], op=ALU.mult)
            vop("pool", "tensor_tensor", [gate_b[s2], x1_b[s3]], [x1_b[s3]], out=x1[s3][:, cs],
                in0=gate[s2][:, cs], in1=x1[s3][:, cs], op=ALU.add)

    def S3a(i):
        s3 = i % 3
        act(junkc[:], x1[s3][:], AF.Square, [x1_b[s3]], [junkc_b, stt_b[s3]],
            accum_out=stt[s3][:, 4:5])

    def S3b(i):
        s2, s3 = i % 2, i % 3
        tok = slice(i * 128, (i + 1) * 128)
        rstd_op(stt[s3][:, 5:6], stt[s3][:, 4:5], 1.0 / D, stt_b[s3])
        vop("dve", "scalar_tensor_tensor", [x1_b[s3], stt_b[s3], gfin_b], [yo_b[s2]], out=yo[s2][:],
            in0=x1[s3][:], scalar=stt[s3][:, 5:6], in1=gfin[:], op0=ALU.mult, op1=ALU.mult)
        dma("sp", out_d[tok, :], yo[s2][:], [yo_b[s2]], [])

    for t in range(-1, NT + 1):
        if 0 <= t + 1 < NT:
            OPX(t + 1)
        if 0 <= t < NT:
            TRE(t)
        if 0 <= t - 1 < NT:
            S3a(t - 1)
        if 0 <= t < NT:
            S2(t)
        if 0 <= t + 1 < NT:
            CST(t + 1)
        if 0 <= t - 1 < NT:
            S3b(t - 1)
    stC.close()
    top.close()
    P.emit(nc)
    return nc, dumps


def _host_inputs(S, x, p, w_in, g_mix, g_out_sb, g_out_mb, w_out, w_ple, g_ple, w_ple_gate, g_final):
    c = _consts(S)
    f = np.float32
    shared = {
        "w_in": np.ascontiguousarray(w_in[0], f),
        "w_out": np.ascontiguousarray(w_out[0], f),
        "w_ple": np.ascontiguousarray(w_ple[0], f),
        "w_gate": np.ascontiguousarray(w_ple_gate[0], f),
        "gmix_bc": np.ascontiguousarray(np.broadcast_to(np.asarray(g_mix[0], f)[None, :], (128, D))),
        "gfin_bc": np.ascontiguousarray(np.broadcast_to(np.asarray(g_final, f)[None, :], (128, D))),
        "gout_pc": np.ascontiguousarray(
            np.concatenate([np.asarray(g_out_sb[0], f), np.asarray(g_out_mb[0], f)]).reshape(8, 128).T),
        "gple_pc": np.ascontiguousarray(np.asarray(g_ple[0], f).reshape(8, 128).T),
    }
    shared.update(c)
    maps = []
    for b in range(x.shape[0]):
        m = dict(shared)
        m["x"] = np.ascontiguousarray(x[b], f)
        m["p"] = np.ascontiguousarray(p[0, b], f)
        maps.append(m)
    return maps


_CACHE = {}


def kernel(x, p, w_in, g_mix, g_out_sb, g_out_mb, w_out, w_ple, g_ple, w_ple_gate, g_final):
    x = np.asarray(x)
    B, S, _ = x.shape
    if S not in _CACHE:
        _CACHE[S] = build(S)[0]
    nc = _CACHE[S]
    maps = _host_inputs(S, x, np.asarray(p), np.asarray(w_in), np.asarray(g_mix), np.asarray(g_out_sb),
                        np.asarray(g_out_mb), np.asarray(w_out), np.asarray(w_ple), np.asarray(g_ple),
                        np.asarray(w_ple_gate), np.asarray(g_final))
    res = run_bass_kernel_spmd(nc, maps, core_ids=list(range(B)))
    return np.stack([np.asarray(r["out"], np.float32) for r in res.results], axis=0)
```

```python
from contextlib import ExitStack

import numpy as np
import ml_dtypes
import concourse.bass as bass
import concourse.mybir as mybir
from concourse.bass_utils import run_bass_kernel_spmd

F32 = mybir.dt.float32
BF16 = mybir.dt.bfloat16
AF = mybir.ActivationFunctionType
ALU = mybir.AluOpType
AX = mybir.AxisListType

D = 1024
DPLE = 256
EPS = 1e-6
NEG = -30000.0
ENGS = ("pe", "act", "dve", "pool", "sp")


class Buf:
    __slots__ = ("name", "w", "r")

    def __init__(self, name=""):
        self.name = name
        self.w = None
        self.r = {}


class Prog:
    def __init__(self, n_dma_sp=12, n_dma_pool=6):
        self.ops = {e: [] for e in ENGS}
        self.cnt = {e: 0 for e in ENGS}
        self.seen = {e: {} for e in ENGS}
        self.dma_pool = {"sp": [("dma", "sp", i) for i in range(n_dma_sp)],
                         "pool": [("dma", "pool", i) for i in range(n_dma_pool)]}
        self.dma_rr = {"sp": 0, "pool": 0}
        self.dma_cnt = {}
        for q in self.dma_pool.values():
            for k in q:
                self.dma_cnt[k] = 0

    def op(self, eng, fn, reads=(), writes=(), dma=False):
        deps = {}

        def add(ev, kind):
            if ev is None:
                return
            key, val = ev
            if key == eng and eng == "pe":
                return
            if deps.get(key, 0) < val:
                deps[key] = val

        for b in reads:
            add(b.w, "raw")
        for b in writes:
            add(b.w, "waw")
            for key, val in b.r.items():
                add((key, val), "war")
        if dma:
            pool = self.dma_pool[eng]
            k = pool[self.dma_rr[eng] % len(pool)]
            self.dma_rr[eng] += 1
            prev = self.dma_cnt[k]
            if prev > 0 and deps.get(k, 0) < prev:
                deps[k] = prev
            self.dma_cnt[k] = prev + 16
            ev = (k, prev + 16)
        else:
            self.cnt[eng] += 1
            ev = (eng, self.cnt[eng])
        seen = self.seen[eng]
        waits = []
        for key, val in deps.items():
            if seen.get(key, 0) < val:
                waits.append((key, val))
                seen[key] = val
        self.ops[eng].append((fn, waits, ev, dma))
        for b in reads:
            if b.r.get(ev[0], 0) < ev[1]:
                b.r[ev[0]] = ev[1]
        for b in writes:
            b.w = ev
            b.r = {}
        return ev

    def alias(self, new_bufs, old_bufs):
        merged = {}
        for b in old_bufs:
            evs = list(b.r.items())
            if b.w is not None:
                evs.append(b.w)
            for key, val in evs:
                if merged.get(key, 0) < val:
                    merged[key] = val
        for b in new_bufs:
            for key, val in merged.items():
                if b.r.get(key, 0) < val:
                    b.r[key] = val

    def emit(self, nc):
        sem_keys = list(ENGS[:4]) + [k for q in self.dma_pool.values() for k in q]
        with ExitStack() as st:
            sems = {}
            for k in sem_keys:
                nm = k if isinstance(k, str) else "d_%s_%d" % (k[1], k[2])
                sems[k] = st.enter_context(nc.semaphore("s_" + nm))
            final_waits = [(k, v) for k, v in self.dma_cnt.items() if v > 0]
            block = st.enter_context(nc.Block())

            def replay(name, e):
                for fn, waits, ev, dma in self.ops[name]:
                    for key, val in waits[:-1]:
                        e.wait_ge(sems[key], val)
                    ins = fn(e)
                    if waits:
                        ins._wait_ge(sems[waits[-1][0]], waits[-1][1])
                    ins.then_inc(sems[ev[0]], 16 if dma else 1)
                if name == "sp":
                    for key, val in final_waits:
                        e.wait_ge(sems[key], val)

            @block.tensor
            def _(e):
                replay("pe", e)

            @block.scalar
            def _(e):
                replay("act", e)

            @block.vector
            def _(e):
                replay("dve", e)

            @block.gpsimd
            def _(e):
                replay("pool", e)

            @block.sync
            def _(e):
                replay("sp", e)


def _consts(S):
    bf = ml_dtypes.bfloat16
    NT = S // 128
    i = np.arange(128)
    c = {}
    c["ident"] = np.eye(128, dtype=np.float32).astype(bf)
    c["negtri"] = (-(i[:, None] >= i[None, :]).astype(np.float32)).astype(bf)
    c["negident"] = (-np.eye(128, dtype=np.float32)).astype(bf)
    c["ones"] = np.ones((128, 128), np.float32).astype(bf)
    c["negones"] = (-np.ones((128, 128), np.float32)).astype(bf)
    c["masksb"] = np.where(i[:, None] < i[None, :], 0.0, NEG).astype(np.float32).astype(bf)
    c["maskmb"] = np.where(i[:, None] <= i[None, :], 0.0, NEG).astype(np.float32).astype(bf)
    c["onesf"] = np.ones((128, 64), np.float32)
    pos = np.arange(S)
    hi = (pos // 256) * 256.0
    lo = (pos % 256) * 1.0
    kaug = np.zeros((8, 20, S), np.float32)
    qaug = np.zeros((8, 4, S), np.float32)
    for h in range(8):
        sl = 2.0 ** (-8.0 * (h + 1) / 8.0)
        for n in range(16):
            kaug[h, n] = (pos // 256 == n)
        kaug[h, 16] = 1.0
        kaug[h, 17] = 1.0
        kaug[h, 18] = sl * hi
        kaug[h, 19] = sl * lo
        qaug[h, 0] = -sl * hi
        qaug[h, 1] = -sl * lo
        qaug[h, 2] = 1.0
        qaug[h, 3] = 1.0
    c["kaug"] = kaug.astype(bf)
    c["qaug"] = qaug.astype(bf)
    gmask = np.zeros((NT, 16), np.float32)
    negcand = np.zeros((NT, 16), np.float32)
    fixed = np.zeros((NT, 16), np.float32)
    for ti in range(NT):
        qb = ti // 2
        for n in range(16):
            if n < qb:
                negcand[ti, n] = NEG
            else:
                gmask[ti, n] = -1e30
            if n > qb:
                fixed[ti, n] = NEG
    c["gmask"] = np.broadcast_to(gmask.reshape(1, NT * 16), (128, NT * 16)).astype(bf)
    c["negcand"] = np.broadcast_to(negcand.reshape(1, NT * 16), (128, NT * 16)).astype(bf)
    c["fixed"] = np.broadcast_to(fixed.reshape(1, NT * 16), (128, NT * 16)).astype(bf)
    return c


def build(S, dbg=None, stop_after=None):
    dbg = set(dbg or ())
    if "qk" in dbg:
        dbg |= {"T0", "T1", "T2", "T3", "vt"}
    NT = S // 128
    NQ = S // 512
    nc = bass.Bass("TRN2", target_bir_lowering=False)
    P = Prog()

    def din(name, shape, dt=F32):
        return nc.dram_tensor(name, list(shape), dt, kind="ExternalInput").ap()

    x_d = din("x", [S, D])
    p_d = din("p", [S, DPLE])
    w_in_d = din("w_in", [D, 4 * D])
    w_out_d = din("w_out", [D, D])
    w_ple_d = din("w_ple", [DPLE, D])
    w_gate_d = din("w_gate", [D, D])
    gmix_d = din("gmix_bc", [128, D])
    gfin_d = din("gfin_bc", [128, D])
    gout_d = din("gout_pc", [128, 8])
    gple_d = din("gple_pc", [128, 8])
    ident_d = din("ident", [128, 128], BF16)
    negtri_d = din("negtri", [128, 128], BF16)
    ones_d = din("ones", [128, 128], BF16)
    negones_d = din("negones", [128, 128], BF16)
    negident_d = din("negident", [128, 128], BF16)
    masksb_d = din("masksb", [128, 128], BF16)
    maskmb_d = din("maskmb", [128, 128], BF16)
    onesf_d = din("onesf", [128, 64])
    kaug_d = din("kaug", [8, 20, S], BF16)
    qaug_d = din("qaug", [8, 4, S], BF16)
    gmask_d = din("gmask", [128, NT * 16], BF16)
    negcand_d = din("negcand", [128, NT * 16], BF16)
    fixed_d = din("fixed", [128, NT * 16], BF16)
    out_d = nc.dram_tensor("out", [S, D], F32, kind="ExternalOutput").ap()

    dumps = {}
    top = ExitStack()

    def sb(name, shape, dt, st=top):
        return st.enter_context(nc.sbuf_tensor("sb_" + name, list(shape), dt))

    def dump(name, src_ap, shape, dt, reads):
        if name not in dbg:
            return
        t = nc.dram_tensor("dbg_" + name, list(shape), dt, kind="ExternalOutput").ap()
        dumps[name] = t
        P.op("sp", lambda e, t=t, s=src_ap: e.dma_start(out=t, in_=s), reads=reads, dma=True)

    def mm(out, lhsT, rhs, start, reads, writes, stop=True):
        P.op("pe", lambda e: e.matmul(out, lhsT, rhs, start=start, stop=stop,
                                      skip_group_check=True), reads=reads, writes=writes)

    def tr(out, in_, reads, writes):
        P.op("pe", lambda e: e.transpose(out=out, in_=in_, identity=ident[:]),
             reads=list(reads) + [cbuf], writes=writes)

    def act(out, in_, func, reads, writes, **kw):
        P.op("act", lambda e: e.activation(out=out, in_=in_, func=func, **kw),
             reads=reads, writes=writes)

    def dma(eng, out, in_, reads, writes):
        P.op(eng, lambda e: e.dma_start(out=out, in_=in_), reads=reads, writes=writes, dma=True)

    def vop(eng, name, reads, writes, **kw):
        P.op(eng, lambda e: getattr(e, name)(**kw), reads=reads, writes=writes)

    def rstd_op(dst, src, scale, b):
        act(dst, src, AF.Sqrt, [b, cbuf], [b], scale=scale, bias=epsc[0:dst.shape[0], 0:1])
        vop("dve", "reciprocal", [b], [b], out=dst, in_=dst)

    def copy_any(k, out, in_, reads, writes):
        if k % 2 == 0:
            P.op("act", lambda e: e.copy(out=out, in_=in_), reads=reads, writes=writes)
        else:
            P.op("dve", lambda e: e.tensor_copy(out=out, in_=in_), reads=reads, writes=writes)

    ident = sb("ident", [128, 128], BF16)
    negtri = sb("negtri", [128, 128], BF16)
    ones = sb("ones", [128, 128], BF16)
    negones = sb("negones", [128, 128], BF16)
    negident = sb("negident", [128, 128], BF16)
    masksb = sb("masksb", [128, 128], BF16)
    maskmb = sb("maskmb", [128, 128], BF16)
    onesf = sb("onesf", [128, 64], F32)
    gout = sb("gout", [128, 8], F32)
    gple = sb("gple", [128, 8], F32)
    epsc = sb("epsc", [128, 1], F32)
    cbuf = Buf("consts")
    P.op("dve", lambda e: e.memset(epsc[:], EPS), writes=[cbuf])
    for t, d in ((ident, ident_d), (negtri, negtri_d), (ones, ones_d), (negones, negones_d),
                 (negident, negident_d), (masksb, masksb_d), (maskmb, maskmb_d), (onesf, onesf_d), (gout, gout_d),
                 (gple, gple_d)):
        dma("sp", t[:], d, [], [cbuf])

    gz = sb("gz", [128, 8, S], BF16)
    gz_b = [[Buf("gz%d_%d" % (pr, qt)) for qt in range(NQ)] for pr in range(8)]
    ssq = sb("ssq", [128, 8, NT], F32)
    ssq_b = Buf("ssq")

    banks = [top.enter_context(nc.psum_tensor("bank%d" % i, [128, 512], F32)) for i in range(8)]
    bank_b = [Buf("bank%d" % i) for i in range(8)]

    def bank_bf(i):
        return banks[i][:].bitcast(BF16)

    stB = ExitStack()
    hT = sb("hT", [128, 8, S], BF16, stB)
    hT_b = [Buf("hT%d" % i) for i in range(NT)]

    stA = ExitStack()
    gmix = sb("gmix", [128, D], F32, stA)
    gmix_b = Buf("gmix")
    dma("sp", gmix[:], gmix_d, [], [gmix_b])
    xt = [sb("xt%d" % i, [128, D], F32, stA) for i in range(2)]
    xt_b = [Buf("xt%d" % i) for i in range(2)]
    junk = sb("junkA", [128, D], BF16, stA)
    junk_b = Buf("junkA")
    hb = [sb("hb%d" % i, [128, D], BF16, stA) for i in range(2)]
    hb_b = [Buf("hb%d" % i) for i in range(2)]
    st1 = [sb("st1_%d" % i, [128, 2], F32, stA) for i in range(2)]
    st1_b = [Buf("st1_%d" % i) for i in range(2)]
    phaseA_bufs = [gmix_b, junk_b] + xt_b + hb_b + st1_b

    def A_norm(i):
        s = i % 2
        dma("sp", xt[s][:], x_d[i * 128:(i + 1) * 128, :], [], [xt_b[s]])
        act(junk[:], xt[s][:], AF.Square, [xt_b[s]], [junk_b, st1_b[s]], accum_out=st1[s][:, 0:1])
        rstd_op(st1[s][:, 1:2], st1[s][:, 0:1], 1.0 / D, st1_b[s])
        vop("dve", "scalar_tensor_tensor", [xt_b[s], st1_b[s], gmix_b], [hb_b[s]],
            out=hb[s][:], in0=xt[s][:], scalar=st1[s][:, 1:2], in1=gmix[:],
            op0=ALU.mult, op1=ALU.mult)

    def A_tr(i):
        s = i % 2
        bk = 6 + (i % 2)
        pT = bank_bf(bk)
        for c in range(8):
            tr(pT[:, c * 128:(c + 1) * 128], hb[s][:, c * 128:(c + 1) * 128], [hb_b[s]], [bank_b[bk]])
        copy_any(i, hT[:, :, i * 128:(i + 1) * 128], pT.rearrange("p (c t) -> p c t", c=8),
                 [bank_b[bk]], [hT_b[i]])

    for i in range(NT + 1):
        if i < NT:
            A_norm(i)
        if i >= 1:
            A_tr(i - 1)
    dump("hT", hT[:], [128, 8, S], BF16, hT_b)
    stA.close()
    if stop_after == "A":
        P.emit(nc)
        return nc, dumps

    T = [sb("T%d" % i, [128, S], BF16, stB) for i in range(4)]
    T_b = [Buf("T%d" % i) for i in range(4)]
    P.alias(T_b, phaseA_bufs)
    vt = sb("vt", [128, NT, 130], BF16, stB)
    vt_b = Buf("vt")
    wbuf = [sb("wbuf0", [128, 8, 4, 128], BF16, stB)] * 2
    wbuf_b = [Buf("wbuf0")] * 2
    scr = sb("scr", [128, 3584], F32, stB)
    esb = [scr[:, 0:512], scr[:, 512:1024]]
    esb_b = [Buf("esb%d" % i) for i in range(2)]
    spb = [scr[:, 1024:1280].bitcast(BF16), scr[:, 1280:1536].bitcast(BF16)]
    spb_b = [Buf("spb%d" % i) for i in range(2)]
    rrow = [scr[:, 1536:1792].bitcast(BF16), scr[:, 1792:2048].bitcast(BF16)]
    rrow_b = [Buf("rrow%d" % i) for i in range(2)]
    sb_only = esb_b + spb_b + rrow_b
    rden = [scr[:, 0:512], scr[:, 2560:3072], scr[:, 3072:3584]]
    rden_b = [Buf("rden%d" % i) for i in range(3)]
    bcsb = scr[0:64, 512:1024]
    bcsb_b = Buf("bcsb")
    gsb = scr[:, 1024:1024 + NT * 16].rearrange("p (t n) -> p t n", n=16)
    gsb_b = Buf("gsb")
    m8 = scr[:, 1536:1536 + NT * 8].rearrange("p (t n) -> p t n", n=8)
    m8_b = Buf("m8")
    biasq = scr[:, 1792:2048].bitcast(BF16)[:, 0:NT * 16].rearrange("p (t n) -> p t n", n=16)
    biasq_b = Buf("biasq")
    biasT = scr[0:16, 2048:2560].bitcast(BF16)
    biasT_b = Buf("biasT")
    mb_only = rden_b + [bcsb_b, gsb_b, m8_b, biasq_b, biasT_b]
    ptb = [sb("ptb%d" % i, [128, 512], BF16, stB) for i in range(2)]
    ptb_b = [Buf("ptb%d" % i) for i in range(2)]
    opair = [sb("opair%d" % i, [128, 512], F32, stB) for i in range(2)]
    opair_b = [Buf("opair%d" % i) for i in range(2)]
    ostage = [sb("ostage0", [64, 512], F32, stB)] * 2
    ostage_b = [Buf("ostage0")] * 2
    osq = sb("osq", [128, 512], BF16, stB)
    osq_b = Buf("osq")
    gmask = sb("gmask", [128, NT * 16], BF16, stB)
    negcand = sb("negcand", [128, NT * 16], BF16, stB)
    fixed = sb("fixed", [128, NT * 16], BF16, stB)
    gconst_b = Buf("gconst")
    ksf = sb("ksf", [64, 16], F32, stB)
    ksb = sb("ksb", [64, 16], BF16, stB)
    ks_b = Buf("ks")
    P.alias([vt_b, gconst_b, ks_b, osq_b, wbuf_b[0], opair_b[0], opair_b[1], ostage_b[0]]
            + ptb_b + sb_only, phaseA_bufs)
    for t, d in ((gmask, gmask_d), (negcand, negcand_d), (fixed, fixed_d)):
        dma("sp", t[:], d, [], [gconst_b])
    vop("pool", "memset", [], [vt_b], ap=vt[:], constant=1.0)
    vop("dve", "memset", [], [ks_b], ap=ksb[:], constant=0.0)

    w_in_v = w_in_d.rearrange("(c p) n -> p c n", p=128)

    def load_w(pr):
        G, j = pr // 4, pr % 4
        wb = wbuf[pr % 2]
        for t in range(4):
            col0 = G * 2048 + t * 512 + j * 128
            dma("pool", wb[:, :, t, :], w_in_v[:, :, col0:col0 + 128], [], [wbuf_b[pr % 2]])

    def in_proj(pr):
        G = pr // 4
        wb = wbuf[pr % 2]
        wb_b = wbuf_b[pr % 2]
        rot = [6, 7, 0, 1, 2]
        k = [0]

        def nb():
            k[0] += 1
            return rot[k[0] % len(rot)]

        for t, scale in ((0, 0.125), (1, None)):
            for tq in range(NQ):
                cols = slice(tq * 512, (tq + 1) * 512)
                ti_ = t if G == 0 else 2 * t
                groups = [(slice(0, 128), T[ti_], T_b[ti_], 128)]
                for wc, dst, dst_b, M in groups:
                    b = nb()
                    for c in range(8):
                        mm(banks[b][0:M, :], wb[:, c, t, wc], hT[:, c, cols], c == 0,
                           [wb_b] + hT_b[tq * 4:(tq + 1) * 4], [bank_b[b]], stop=(c == 7))
                    if scale is not None:
                        if k[0] % 2 == 0:
                            act(dst[0:M, cols], banks[b][0:M, :], AF.Copy, [bank_b[b]], [dst_b],
                                scale=scale)
                        else:
                            vop("dve", "tensor_scalar", [bank_b[b]], [dst_b], out=dst[0:M, cols],
                                in0=banks[b][0:M, :], scalar1=scale, scalar2=None, op0=ALU.mult)
                    else:
                        copy_any(k[0], dst[0:M, cols], banks[b][0:M, :], [bank_b[b]], [dst_b])
        if G == 1:
            for t in range(2):
                dma("sp", T[2 * t + 1][0:64, :], T[2 * t][64:128, :], [T_b[2 * t]], [T_b[2 * t + 1]])
        for tq in range(NQ):
            cols = slice(tq * 512, (tq + 1) * 512)
            b = nb()
            for c in range(8):
                mm(banks[b][:, :], wb[:, c, 3, :], hT[:, c, cols], c == 0,
                   [wb_b] + hT_b[tq * 4:(tq + 1) * 4], [bank_b[b]], stop=(c == 7))
            act(gz[:, pr, cols], banks[b][:, :], AF.Silu, [bank_b[b]], [gz_b[pr][tq]])
        for g in range(NT // 4):
            b = nb()
            for tt in range(4):
                ti = g * 4 + tt
                for c in range(8):
                    mm(banks[b][:, tt * 128:(tt + 1) * 128], hT[:, c, ti * 128:(ti + 1) * 128],
                       wb[:, c, 2, :], c == 0, [wb_b, hT_b[ti]], [bank_b[b]], stop=(c == 7))
            bv = banks[b][:].rearrange("p (t n) -> p t n", t=4)
            P.op("act", lambda e, g=g, bv=bv: e.copy(out=vt[:, g * 4:(g + 1) * 4, 0:64],
                                                     in_=bv[:, :, 0:64]),
                 reads=[bank_b[b]], writes=[vt_b])
            P.op("dve", lambda e, g=g, bv=bv: e.tensor_copy(out=vt[:, g * 4:(g + 1) * 4, 65:129],
                                                            in_=bv[:, :, 64:128]),
                 reads=[bank_b[b]], writes=[vt_b])

    def moba_gate(h, qT, qT_b, kT, kT_b):
        dma("sp", kT[64:84, :], kaug_d[h], [], [kT_b])
        dma("sp", qT[80:84, :], qaug_d[h], [], [qT_b])
        NB = S // 256
        vop("dve", "tensor_reduce", [kT_b], [ks_b], out=ksf[:, 0:NB],
            in_=kT[0:64, :].rearrange("p (n s) -> p n s", s=256), axis=AX.X, op=ALU.add)
        vop("dve", "tensor_copy", [ks_b], [ks_b], out=ksb[:, 0:NB], in_=ksf[:, 0:NB])
        gb = 6
        for ti in range(NT):
            mm(banks[gb][:, ti * 16:(ti + 1) * 16], qT[0:64, ti * 128:(ti + 1) * 128], ksb[:, :],
               True, [qT_b, ks_b], [bank_b[gb]])
        vop("dve", "tensor_tensor", [bank_b[gb], gconst_b], [gsb_b],
            out=gsb, in0=banks[gb][:, 0:NT * 16].rearrange("p (t n) -> p t n", n=16),
            in1=gmask[:].rearrange("p (t n) -> p t n", n=16), op=ALU.add)
        for ti in range(NT):
            vop("dve", "max", [gsb_b], [m8_b], out=m8[:, ti, :], in_=gsb[:, ti, :])
        vop("dve", "tensor_tensor", [gsb_b, m8_b], [gsb_b], out=gsb, in0=gsb,
            in1=m8[:, :, 2:3].to_broadcast([128, NT, 16]), op=ALU.is_lt)
        vop("dve", "tensor_tensor", [gsb_b, gconst_b], [gsb_b], out=gsb, in0=gsb,
            in1=negcand[:].rearrange("p (t n) -> p t n", n=16), op=ALU.mult)
        vop("dve", "tensor_tensor", [gsb_b, gconst_b], [biasq_b], out=biasq, in0=gsb,
            in1=fixed[:].rearrange("p (t n) -> p t n", n=16), op=ALU.add)
        tb = 7
        pT = bank_bf(tb)
        for g in range((NT + 7) // 8):
            n = min(8, NT - g * 8)
            for tt in range(n):
                ti = g * 8 + tt
                tr(pT[0:16, tt * 128:(tt + 1) * 128], biasq[:, ti, :], [biasq_b], [bank_b[tb]])
            copy_any(g, biasT[:, 0:n * 128], pT[0:16, 0:n * 128], [bank_b[tb]], [biasT_b])
            dma("sp", qT[64:80, g * 1024:g * 1024 + n * 128], biasT[:, 0:n * 128], [biasT_b], [qT_b])

    ZB = [0, 1, 2]
    OB = [3, 4, 7]
    RB = 5
    MB = 6

    def attention(pr):
        G = pr // 4
        heads = []
        for hh in range(2):
            if G == 0:
                heads.append(dict(q=T[0], q_b=T_b[0], k=T[1], k_b=T_b[1], pb=64 * hh, K=64))
            else:
                heads.append(dict(q=T[hh], q_b=T_b[hh], k=T[2 + hh], k_b=T_b[2 + hh], pb=0, K=84))
        mask = masksb if G == 0 else maskmb
        steps = []
        gcount = 0
        for qt in (range(NQ) if G == 0 else range(NQ - 1, -1, -1)):
            for hh in range(2):
                kts = list(range(4 * qt + 3, -1, -1))
                for si, kt in enumerate(kts):
                    steps.append(dict(qt=qt, hh=hh, kt=kt, si=si, last=(si == len(kts) - 1),
                                      idx=len(steps), g=gcount))
                gcount += 1
        n = len(steps)

        def geom(st):
            j = st["kt"] - 4 * st["qt"]
            c0 = 128 * j if j > 0 else 0
            return j, c0

        def emit_Z(st):
            hd = heads[st["hh"]]
            j, c0 = geom(st)
            zb = ZB[st["idx"] % 3]
            pb, K = hd["pb"], hd["K"]
            kt, qt = st["kt"], st["qt"]
            mm(banks[zb][:, c0:512], hd["k"][pb:pb + K, kt * 128:(kt + 1) * 128],
               hd["q"][pb:pb + K, qt * 512 + c0:(qt + 1) * 512], True,
               [hd["k_b"], hd["q_b"]], [bank_b[zb]], stop=False)
            if j >= 0:
                mm(banks[zb][:, c0:c0 + 128], ident[:], mask[:], False, [cbuf], [bank_b[zb]],
                   stop=False)

        def emit_A1(st):
            j, c0 = geom(st)
            zb = ZB[st["idx"] % 3]
            s2 = st["idx"] % 2
            act(esb[s2][:, c0:512], banks[zb][:, c0:512], AF.Exp, [bank_b[zb]], [esb_b[s2]])

        def emit_A2(st):
            j, c0 = geom(st)
            s2 = st["idx"] % 2
            act(spb[s2][:, c0:512], esb[s2][:, c0:512], AF.Ln, [esb_b[s2]], [spb_b[s2]], bias=1.0)

        def emit_tri(st):
            j, c0 = geom(st)
            zb = ZB[st["idx"] % 3]
            s2 = st["idx"] % 2
            mm(banks[zb][:, c0:512], negtri[:], spb[s2][:, c0:512], False,
               [cbuf, spb_b[s2]], [bank_b[zb]], stop=False)
            if st["si"] > 0:
                cR = geom(steps[st["idx"] - 1])[1]
                mm(banks[zb][:, cR:512], negident[:], rrow[s2][:, cR:512], False,
                   [cbuf, rrow_b[s2]], [bank_b[zb]], stop=True)
            if not st["last"]:
                mm(banks[RB][:, c0:512], ones[:, :], spb[s2][:, c0:512], st["si"] == 0,
                   [cbuf, spb_b[s2]], [bank_b[RB]], stop=False)
                n2 = (st["idx"] + 1) % 2
                vop("dve", "tensor_copy", [bank_b[RB]], [rrow_b[n2]], out=rrow[n2][:, c0:512],
                    in_=banks[RB][:, c0:512])

        def emit_A3(st):
            j, c0 = geom(st)
            zb = ZB[st["idx"] % 3]
            s2 = st["idx"] % 2
            act(ptb[s2][:, c0:512], banks[zb][:, c0:512], AF.Exp, [bank_b[zb]], [ptb_b[s2]])

        def emit_PV(st):
            j, c0 = geom(st)
            s2 = st["idx"] % 2
            hh, qt, kt = st["hh"], st["qt"], st["kt"]
            ob = OB[st["g"] % 3]
            M = 64 if G == 0 else 65
            mm(banks[ob][0:M, c0:512], vt[:, kt, hh * 65:hh * 65 + M], ptb[s2][:, c0:512],
               st["si"] == 0, [vt_b, ptb_b[s2]], [bank_b[ob]], stop=st["last"])
            if st["last"]:
                if G == 0:
                    pending.append([3, "fin", st, ob])
                else:
                    pending.append([1, "ln", st, ob])

        def emit_evac(st, ob):
            hh, qt = st["hh"], st["qt"]
            op_i = qt % 2
            cols = slice(qt * 512, (qt + 1) * 512)
            if hh == 0:
                dst, dst_b = opair[op_i][0:64, :], opair_b[op_i]
            else:
                dst, dst_b = ostage[qt % 2][:, :], ostage_b[qt % 2]
            if G == 0:
                copy_any(qt + hh, dst, banks[ob][0:64, :], [bank_b[ob]], [dst_b])
            else:
                act(bcsb, banks[MB][0:64, :], AF.Exp, [bank_b[MB]], [bcsb_b], scale=-1.0)
                vop("dve", "tensor_tensor", [bank_b[ob], bcsb_b], [dst_b], out=dst,
                    in0=banks[ob][0:64, :], in1=bcsb, op=ALU.mult)
            if hh == 1:
                dma("sp", opair[op_i][64:128, :], ostage[qt % 2][:, :], [ostage_b[qt % 2]],
                    [opair_b[op_i]])
                vop("pool", "tensor_tensor", [opair_b[op_i]], [osq_b], out=osq[:, :],
                    in0=opair[op_i][:, :], in1=opair[op_i][:, :], op=ALU.mult)
                for tt in range(4):
                    mm(banks[MB][:, 508 + tt:509 + tt], osq[:, tt * 128:(tt + 1) * 128],
                       ones[:, 0:1], True, [osq_b, cbuf], [bank_b[MB]])
                vop("dve", "tensor_copy", [bank_b[MB]], [ssq_b], out=ssq[:, pr, qt * 4:(qt + 1) * 4],
                    in_=banks[MB][:, 508:512])
                vop("dve", "tensor_tensor", [opair_b[op_i], gz_b[pr][qt]], [gz_b[pr][qt]],
                    out=gz[:, pr, cols], in0=opair[op_i][:, :], in1=gz[:, pr, cols], op=ALU.mult)

        pending = []

        def run_stage(kind, st, ob):
            ri = st["g"] % 3
            if kind == "ln":
                act(rden[ri][64:65, :], banks[ob][64:65, :], AF.Ln, [bank_b[ob]], [rden_b[ri]])
                pending.append([2, "bc", st, ob])
            elif kind == "bc":
                mm(banks[MB][0:64, :], onesf[64:65, 0:64], rden[ri][64:65, :], True,
                   [cbuf, rden_b[ri]], [bank_b[MB]])
                pending.append([2, "fin", st, ob])
            else:
                emit_evac(st, ob)

        def flush(all_=False):
            while True:
                for pe_ in list(pending):
                    pe_[0] -= 1
                    if pe_[0] <= 0 or all_:
                        pending.remove(pe_)
                        run_stage(pe_[1], pe_[2], pe_[3])
                if not (all_ and pending):
                    break

        if G == 0:
            emit_Z(steps[0])
            for i in range(n + 2):
                if 0 <= i - 1 < n:
                    emit_tri(steps[i - 1])
                if 0 <= i - 2 < n:
                    emit_PV(steps[i - 2])
                if i + 1 < n:
                    emit_Z(steps[i + 1])
                if i < n:
                    emit_A1(steps[i])
                if 0 <= i - 1 < n:
                    emit_A3(steps[i - 1])
                if i < n:
                    emit_A2(steps[i])
                flush()
        else:
            emit_Z(steps[0])
            if n > 1:
                emit_Z(steps[1])
            for i in range(n + 1):
                if i < n:
                    emit_A3(steps[i])
                if 0 <= i - 1 < n:
                    emit_PV(steps[i - 1])
                if i + 2 < n:
                    emit_Z(steps[i + 2])
                flush()
        flush(True)

    npairs = 8 if stop_after is None else int(stop_after[1:]) if stop_after.startswith("B") else 8
    load_w(0)
    for pr in range(npairs):
        if pr == 4:
            P.alias(mb_only, sb_only)
        in_proj(pr)
        if pr + 1 < 8:
            load_w(pr + 1)
        if pr // 4 == 1:
            for hh in range(2):
                moba_gate((pr % 4) * 2 + hh, T[hh], T_b[hh], T[2 + hh], T_b[2 + hh])
        if "qk" in dbg and pr == int(next(iter(d for d in dbg if d.startswith("pair="))).split("=")[1]):
            for i in range(2 if pr < 4 else 4):
                dump("T%d" % i, T[i][:] if pr < 4 else T[i][0:84, :], [128 if pr < 4 else 84, S], BF16,
                     [T_b[i]])
            dump("vt", vt[:], [128, NT, 130], BF16, [vt_b])
        attention(pr)
    dump("gz", gz[:, 0:npairs, :], [128, npairs, S], BF16, [b for l in gz_b for b in l])
    dump("ssq", ssq[:, 0:npairs, :], [128, npairs, NT], F32, [ssq_b])
    phaseB_bufs = (hT_b + T_b + [vt_b, gconst_b, ks_b, osq_b, wbuf_b[0], ostage_b[0]] + ptb_b + opair_b
                   + sb_only + mb_only)
    stB.close()
    if stop_after is not None and stop_after.startswith("B"):
        P.emit(nc)
        return nc, dumps

    stC = ExitStack()
    wo = sb("wo", [128, 8, D], BF16, stC)
    wg = sb("wg", [128, 8, D], BF16, stC)
    wp = sb("wp", [128, 2, D], BF16, stC)
    wo_b, wg_b, wp_b = Buf("wo"), Buf("wg"), Buf("wp")
    wst = [sb("wst%d" % i, [128, D], F32, stC) for i in range(2)]
    wst_b = [Buf("wst%d" % i) for i in range(2)]
    gfin = sb("gfin", [128, D], F32, stC)
    gfin_b = Buf("gfin")

    def mk(name, n, shape, dt):
        return ([sb("%s%d" % (name, i), shape, dt, stC) for i in range(n)],
                [Buf("%s%d" % (name, i)) for i in range(n)])

    xc, xc_b = mk("xc", 3, [128, D], F32)
    pc, pc_b = mk("pc", 3, [128, DPLE], F32)
    pcb, pcb_b = mk("pcb", 2, [128, DPLE], BF16)
    pT_sb, pT_b = mk("pTsb", 2, [128, 2, 128], BF16)
    x1, x1_b = mk("x1_", 3, [128, D], F32)
    x1b, x1b_b = mk("x1b_", 2, [128, D], BF16)
    x1T, x1T_b = mk("x1T_", 2, [128, 8, 128], BF16)
    gate, gate_b = mk("gate", 2, [128, D], F32)
    yo, yo_b = mk("yo", 2, [128, D], F32)
    stt, stt_b = mk("stt", 3, [128, 8], F32)
    junkc = sb("junkc", [128, D], BF16, stC)
    junkc_b = Buf("junkc")
    newC = ([wo_b, wg_b, wp_b, gfin_b, junkc_b] + wst_b + xc_b + pc_b + pcb_b + pT_b + x1_b + x1b_b
            + x1T_b + gate_b + yo_b + stt_b)
    P.alias(newC, phaseB_bufs)

    dma("sp", gfin[:], gfin_d, [], [gfin_b])
    w_out_v = w_out_d.rearrange("(c p) n -> p c n", p=128)
    w_gate_v = w_gate_d.rearrange("(c p) n -> p c n", p=128)
    k = 0
    for (wv, wt, wt_b, gv) in ((w_out_v, wo, wo_b, gout), (w_gate_v, wg, wg_b, gple)):
        for c in range(8):
            s = k % 2
            dma("sp", wst[s][:], wv[:, c, :], [], [wst_b[s]])
            eng = "dve" if k % 2 == 0 else "act"
            if eng == "dve":
                vop("dve", "tensor_scalar", [wst_b[s], cbuf], [wt_b], out=wt[:, c, :], in0=wst[s][:],
                    scalar1=gv[:, c:c + 1], scalar2=None, op0=ALU.mult)
            else:
                act(wt[:, c, :], wst[s][:], AF.Copy, [wst_b[s], cbuf], [wt_b], scale=gv[:, c:c + 1])
            k += 1
    dma("pool", wp[:], w_ple_d.rearrange("(c p) n -> p c n", p=128), [], [wp_b])

    rg = sb("rg", [128, NT, 2], F32, stC)
    rg_b = Buf("rg")
    P.alias([rg_b], phaseB_bufs)
    vop("dve", "tensor_reduce", [ssq_b], [rg_b], out=rg[:],
        in_=ssq[:].rearrange("p (g c) t -> p t g c", g=2), axis=AX.X, op=ALU.add)
    rstd_op(rg[:].rearrange("p t g -> p (t g)"), rg[:].rearrange("p t g -> p (t g)"), 1.0 / 512, rg_b)

    def LOAD(i):
        s3 = i % 3
        tok = slice(i * 128, (i + 1) * 128)
        dma("sp", xc[s3][:], x_d[tok, :], [], [xc_b[s3]])
        dma("sp", pc[s3][:], p_d[tok, :], [], [pc_b[s3]])

    def OPX(i):
        s2, s3 = i % 2, i % 3
        tok = slice(i * 128, (i + 1) * 128)
        for g in range(2):
            for hf in range(2):
                b = g * 2 + hf
                for c4 in range(4):
                    c = g * 4 + c4
                    mm(banks[b][:, :], gz[:, c, tok], wo[:, c, hf * 512:(hf + 1) * 512], c4 == 0,
                       gz_b[c] + [wo_b], [bank_b[b]], stop=(c4 == 3))
        for hf in range(2):
            cs = slice(hf * 512, (hf + 1) * 512)
            vop("dve", "scalar_tensor_tensor", [bank_b[hf], rg_b, xc_b[s3]], [x1_b[s3]],
                out=x1[s3][:, cs], in0=banks[hf][:, :], scalar=rg[:, i, 0:1], in1=xc[s3][:, cs],
                op0=ALU.mult, op1=ALU.add)
            vop("dve", "scalar_tensor_tensor", [bank_b[2 + hf], rg_b, x1_b[s3]], [x1_b[s3]],
                out=x1[s3][:, cs], in0=banks[2 + hf][:, :], scalar=rg[:, i, 1:2], in1=x1[s3][:, cs],
                op0=ALU.mult, op1=ALU.add)
        vop("pool", "tensor_copy", [pc_b[s3]], [pcb_b[s2]], out=pcb[s2][:], in_=pc[s3][:])
        pT5 = bank_bf(5)
        for c in range(2):
            tr(pT5[:, c * 128:(c + 1) * 128], pcb[s2][:, c * 128:(c + 1) * 128], [pcb_b[s2]], [bank_b[5]])
        copy_any(1, pT_sb[s2][:], pT5[:, 0:256].rearrange("p (c t) -> p c t", c=2), [bank_b[5]],
                 [pT_b[s2]])

    def CST(i):
        s2, s3 = i % 2, i % 3
        P.op("act", lambda e: e.copy(out=x1b[s2][:], in_=x1[s3][:]), reads=[x1_b[s3]],
             writes=[x1b_b[s2]])
        act(junkc[:], x1[s3][:], AF.Square, [x1_b[s3]], [junkc_b, stt_b[s3]],
            accum_out=stt[s3][:, 2:3])
        rstd_op(stt[s3][:, 3:4], stt[s3][:, 2:3], 1.0 / D, stt_b[s3])

    def TRE(i):
        s2, s3 = i % 2, i % 3
        pT = bank_bf(4)
        for c in range(8):
            tr(pT[:, c * 128:(c + 1) * 128], x1b[s2][:, c * 128:(c + 1) * 128], [x1b_b[s2]], [bank_b[4]])
        copy_any(0, x1T[s2][:], pT.rearrange("p (c t) -> p c t", c=8), [bank_b[4]], [x1T_b[s2]])

    def S2(i):
        s2, s3 = i % 2, i % 3
        for hf in range(2):
            cs = slice(hf * 512, (hf + 1) * 512)
            gb = 6 if hf == 0 else 5
            for c in range(8):
                mm(banks[gb][:, :], x1T[s2][:, c, :], wg[:, c, cs], c == 0, [x1T_b[s2], wg_b],
                   [bank_b[gb]], stop=(c == 7))
            act(gate[s2][:, cs], banks[gb][:, :], AF.Sigmoid, [bank_b[gb], stt_b[s3]], [gate_b[s2]],
                scale=stt[s3][:, 3:4])
            for c in range(2):
                mm(banks[7][:, :], pT_sb[s2][:, c, :], wp[:, c, cs], c == 0, [pT_b[s2], wp_b],
                   [bank_b[7]], stop=(c == 1))
            vop("dve", "tensor_tensor", [bank_b[7], gate_b[s2]], [gate_b[s2]], out=gate[s2][:, cs],
                in0=banks[7][:, :], in1=gate[s2][:, cs], op=ALU.mult)
            vop("pool", "tensor_tensor", [gate_b[s2], x1_b[s3]], [x1_b[s3]], out=x1[s3][:, cs],
                in0=gate[s2][:, cs], in1=x1[s3][:, cs], op=ALU.add)

    def S3a(i):
        s3 = i % 3
        act(junkc[:], x1[s3][:], AF.Square, [x1_b[s3]], [junkc_b, stt_b[s3]],
            accum_out=stt[s3][:, 4:5])

    def S3b(i):
        s2, s3 = i % 2, i % 3
        tok = slice(i * 128, (i + 1) * 128)
        rstd_op(stt[s3][:, 5:6], stt[s3][:, 4:5], 1.0 / D, stt_b[s3])
        vop("dve", "scalar_tensor_tensor", [x1_b[s3], stt_b[s3], gfin_b], [yo_b[s2]], out=yo[s2][:],
            in0=x1[s3][:], scalar=stt[s3][:, 5:6], in1=gfin[:], op0=ALU.mult, op1=ALU.mult)
        dma("sp", out_d[tok, :], yo[s2][:], [yo_b[s2]], [])

    LOAD(0)
    if NT > 1:
        LOAD(1)
    for t in range(-1, NT + 1):
        if 0 <= t + 3 < NT:
            LOAD(t + 3)
        if 0 <= t + 1 < NT:
            OPX(t + 1)
        if 0 <= t < NT:
            TRE(t)
        if 0 <= t - 1 < NT:
            S3a(t - 1)
        if 0 <= t < NT:
            S2(t)
        if 0 <= t + 1 < NT:
            CST(t + 1)
        if 0 <= t - 1 < NT:
            S3b(t - 1)
    stC.close()
    top.close()
    P.emit(nc)
    return nc, dumps


def _host_inputs(S, x, p, w_in, g_mix, g_out_sb, g_out_mb, w_out, w_ple, g_ple, w_ple_gate, g_final):
    c = _consts(S)
    f = np.float32
    shared = {
        "w_in": np.ascontiguousarray(w_in[0], f),
        "w_out": np.ascontiguousarray(w_out[0], f),
        "w_ple": np.ascontiguousarray(w_ple[0], f),
        "w_gate": np.ascontiguousarray(w_ple_gate[0], f),
        "gmix_bc": np.ascontiguousarray(np.broadcast_to(np.asarray(g_mix[0], f)[None, :], (128, D))),
        "gfin_bc": np.ascontiguousarray(np.broadcast_to(np.asarray(g_final, f)[None, :], (128, D))),
        "gout_pc": np.ascontiguousarray(
            np.concatenate([np.asarray(g_out_sb[0], f), np.asarray(g_out_mb[0], f)]).reshape(8, 128).T),
        "gple_pc": np.ascontiguousarray(np.asarray(g_ple[0], f).reshape(8, 128).T),
    }
    shared.update(c)
    maps = []
    for b in range(x.shape[0]):
        m = dict(shared)
        m["x"] = np.ascontiguousarray(x[b], f)
        m["p"] = np.ascontiguousarray(p[0, b], f)
        maps.append(m)
    return maps


_CACHE = {}


def kernel(x, p, w_in, g_mix, g_out_sb, g_out_mb, w_out, w_ple, g_ple, w_ple_gate, g_final):
    x = np.asarray(x)
    B, S, _ = x.shape
    if S not in _CACHE:
        _CACHE[S] = build(S)[0]
    nc = _CACHE[S]
    maps = _host_inputs(S, x, np.asarray(p), np.asarray(w_in), np.asarray(g_mix), np.asarray(g_out_sb),
                        np.asarray(g_out_mb), np.asarray(w_out), np.asarray(w_ple), np.asarray(g_ple),
                        np.asarray(w_ple_gate), np.asarray(g_final))
    res = run_bass_kernel_spmd(nc, maps, core_ids=list(range(B)))
    return np.stack([np.asarray(r["out"], np.float32) for r in res.results], axis=0)
```

```python
from contextlib import ExitStack

import numpy as np
import ml_dtypes
import concourse.bass as bass
import concourse.mybir as mybir
from concourse.bass_utils import run_bass_kernel_spmd

F32 = mybir.dt.float32
BF16 = mybir.dt.bfloat16
AF = mybir.ActivationFunctionType
ALU = mybir.AluOpType
AX = mybir.AxisListType

D = 1024
DPLE = 256
EPS = 1e-6
NEG = -30000.0
ENGS = ("pe", "act", "dve", "pool", "sp")


class Buf:
    __slots__ = ("name", "w", "r")

    def __init__(self, name=""):
        self.name = name
        self.w = None
        self.r = {}


class Prog:
    def __init__(self, n_dma_sp=12, n_dma_pool=6):
        self.ops = {e: [] for e in ENGS}
        self.cnt = {e: 0 for e in ENGS}
        self.seen = {e: {} for e in ENGS}
        self.dma_pool = {"sp": [("dma", "sp", i) for i in range(n_dma_sp)],
                         "pool": [("dma", "pool", i) for i in range(n_dma_pool)]}
        self.dma_rr = {"sp": 0, "pool": 0}
        self.dma_cnt = {}
        for q in self.dma_pool.values():
            for k in q:
                self.dma_cnt[k] = 0

    def op(self, eng, fn, reads=(), writes=(), dma=False):
        deps = {}

        def add(ev, kind):
            if ev is None:
                return
            key, val = ev
            if key == eng and eng == "pe":
                return
            if deps.get(key, 0) < val:
                deps[key] = val

        for b in reads:
            add(b.w, "raw")
        for b in writes:
            add(b.w, "waw")
            for key, val in b.r.items():
                add((key, val), "war")
        if dma:
            pool = self.dma_pool[eng]
            k = pool[self.dma_rr[eng] % len(pool)]
            self.dma_rr[eng] += 1
            prev = self.dma_cnt[k]
            if prev > 0 and deps.get(k, 0) < prev:
                deps[k] = prev
            self.dma_cnt[k] = prev + 16
            ev = (k, prev + 16)
        else:
            self.cnt[eng] += 1
            ev = (eng, self.cnt[eng])
        seen = self.seen[eng]
        waits = []
        for key, val in deps.items():
            if seen.get(key, 0) < val:
                waits.append((key, val))
                seen[key] = val
        self.ops[eng].append((fn, waits, ev, dma))
        for b in reads:
            if b.r.get(ev[0], 0) < ev[1]:
                b.r[ev[0]] = ev[1]
        for b in writes:
            b.w = ev
            b.r = {}
        return ev

    def alias(self, new_bufs, old_bufs):
        merged = {}
        for b in old_bufs:
            evs = list(b.r.items())
            if b.w is not None:
                evs.append(b.w)
            for key, val in evs:
                if merged.get(key, 0) < val:
                    merged[key] = val
        for b in new_bufs:
            for key, val in merged.items():
                if b.r.get(key, 0) < val:
                    b.r[key] = val

    def emit(self, nc):
        sem_keys = list(ENGS[:4]) + [k for q in self.dma_pool.values() for k in q]
        with ExitStack() as st:
            sems = {}
            for k in sem_keys:
                nm = k if isinstance(k, str) else "d_%s_%d" % (k[1], k[2])
                sems[k] = st.enter_context(nc.semaphore("s_" + nm))
            final_waits = [(k, v) for k, v in self.dma_cnt.items() if v > 0]
            block = st.enter_context(nc.Block())

            def replay(name, e):
                for fn, waits, ev, dma in self.ops[name]:
                    for key, val in waits[:-1]:
                        e.wait_ge(sems[key], val)
                    ins = fn(e)
                    if waits:
                        ins._wait_ge(sems[waits[-1][0]], waits[-1][1])
                    ins.then_inc(sems[ev[0]], 16 if dma else 1)
                if name == "sp":
                    for key, val in final_waits:
                        e.wait_ge(sems[key], val)

            @block.tensor
            def _(e):
                replay("pe", e)

            @block.scalar
            def _(e):
                replay("act", e)

            @block.vector
            def _(e):
                replay("dve", e)

            @block.gpsimd
            def _(e):
                replay("pool", e)

            @block.sync
            def _(e):
                replay("sp", e)


def _consts(S):
    bf = ml_dtypes.bfloat16
    NT = S // 128
    i = np.arange(128)
    c = {}
    c["ident"] = np.eye(128, dtype=np.float32).astype(bf)
    c["negtri"] = (-(i[:, None] >= i[None, :]).astype(np.float32)).astype(bf)
    c["negident"] = (-np.eye(128, dtype=np.float32)).astype(bf)
    c["ones"] = np.ones((128, 128), np.float32).astype(bf)
    c["negones"] = (-np.ones((128, 128), np.float32)).astype(bf)
    c["masksb"] = np.where(i[:, None] < i[None, :], 0.0, NEG).astype(np.float32).astype(bf)
    c["maskmb"] = np.where(i[:, None] <= i[None, :], 0.0, NEG).astype(np.float32).astype(bf)
    c["onesf"] = np.ones((128, 64), np.float32)
    pos = np.arange(S)
    hi = (pos // 256) * 256.0
    lo = (pos % 256) * 1.0
    kaug = np.zeros((8, 20, S), np.float32)
    qaug = np.zeros((8, 4, S), np.float32)
    for h in range(8):
        sl = 2.0 ** (-8.0 * (h + 1) / 8.0)
        for n in range(16):
            kaug[h, n] = (pos // 256 == n)
        kaug[h, 16] = 1.0
        kaug[h, 17] = 1.0
        kaug[h, 18] = sl * hi
        kaug[h, 19] = sl * lo
        qaug[h, 0] = -sl * hi
        qaug[h, 1] = -sl * lo
        qaug[h, 2] = 1.0
        qaug[h, 3] = 1.0
    c["kaug"] = kaug.astype(bf)
    c["qaug"] = qaug.astype(bf)
    gmask = np.zeros((NT, 16), np.float32)
    negcand = np.zeros((NT, 16), np.float32)
    fixed = np.zeros((NT, 16), np.float32)
    for ti in range(NT):
        qb = ti // 2
        for n in range(16):
            if n < qb:
                negcand[ti, n] = NEG
            else:
                gmask[ti, n] = -1e30
            if n > qb:
                fixed[ti, n] = NEG
    c["gmask"] = np.broadcast_to(gmask.reshape(1, NT * 16), (128, NT * 16)).astype(bf)
    c["negcand"] = np.broadcast_to(negcand.reshape(1, NT * 16), (128, NT * 16)).astype(bf)
    c["fixed"] = np.broadcast_to(fixed.reshape(1, NT * 16), (128, NT * 16)).astype(bf)
    return c


def build(S, dbg=None, stop_after=None):
    dbg = set(dbg or ())
    if "qk" in dbg:
        dbg |= {"T0", "T1", "T2", "T3", "vt"}
    NT = S // 128
    NQ = S // 512
    nc = bass.Bass("TRN2", target_bir_lowering=False)
    P = Prog()

    def din(name, shape, dt=F32):
        return nc.dram_tensor(name, list(shape), dt, kind="ExternalInput").ap()

    x_d = din("x", [S, D])
    p_d = din("p", [S, DPLE])
    w_in_d = din("w_in", [D, 4 * D])
    w_out_d = din("w_out", [D, D])
    w_ple_d = din("w_ple", [DPLE, D])
    w_gate_d = din("w_gate", [D, D])
    gmix_d = din("gmix_bc", [128, D])
    gfin_d = din("gfin_bc", [128, D])
    gout_d = din("gout_pc", [128, 8])
    gple_d = din("gple_pc", [128, 8])
    ident_d = din("ident", [128, 128], BF16)
    negtri_d = din("negtri", [128, 128], BF16)
    ones_d = din("ones", [128, 128], BF16)
    negones_d = din("negones", [128, 128], BF16)
    negident_d = din("negident", [128, 128], BF16)
    masksb_d = din("masksb", [128, 128], BF16)
    maskmb_d = din("maskmb", [128, 128], BF16)
    onesf_d = din("onesf", [128, 64])
    kaug_d = din("kaug", [8, 20, S], BF16)
    qaug_d = din("qaug", [8, 4, S], BF16)
    gmask_d = din("gmask", [128, NT * 16], BF16)
    negcand_d = din("negcand", [128, NT * 16], BF16)
    fixed_d = din("fixed", [128, NT * 16], BF16)
    out_d = nc.dram_tensor("out", [S, D], F32, kind="ExternalOutput").ap()

    dumps = {}
    top = ExitStack()

    def sb(name, shape, dt, st=top):
        return st.enter_context(nc.sbuf_tensor("sb_" + name, list(shape), dt))

    def dump(name, src_ap, shape, dt, reads):
        if name not in dbg:
            return
        t = nc.dram_tensor("dbg_" + name, list(shape), dt, kind="ExternalOutput").ap()
        dumps[name] = t
        P.op("sp", lambda e, t=t, s=src_ap: e.dma_start(out=t, in_=s), reads=reads, dma=True)

    def mm(out, lhsT, rhs, start, reads, writes, stop=True):
        P.op("pe", lambda e: e.matmul(out, lhsT, rhs, start=start, stop=stop,
                                      skip_group_check=True), reads=reads, writes=writes)

    def tr(out, in_, reads, writes):
        P.op("pe", lambda e: e.transpose(out=out, in_=in_, identity=ident[:]),
             reads=list(reads) + [cbuf], writes=writes)

    def act(out, in_, func, reads, writes, **kw):
        P.op("act", lambda e: e.activation(out=out, in_=in_, func=func, **kw),
             reads=reads, writes=writes)

    def dma(eng, out, in_, reads, writes):
        P.op(eng, lambda e: e.dma_start(out=out, in_=in_), reads=reads, writes=writes, dma=True)

    def vop(eng, name, reads, writes, **kw):
        P.op(eng, lambda e: getattr(e, name)(**kw), reads=reads, writes=writes)

    def rstd_op(dst, src, scale, b):
        act(dst, src, AF.Sqrt, [b, cbuf], [b], scale=scale, bias=epsc[0:dst.shape[0], 0:1])
        vop("dve", "reciprocal", [b], [b], out=dst, in_=dst)

    def copy_any(k, out, in_, reads, writes):
        if k % 2 == 0:
            P.op("act", lambda e: e.copy(out=out, in_=in_), reads=reads, writes=writes)
        else:
            P.op("dve", lambda e: e.tensor_copy(out=out, in_=in_), reads=reads, writes=writes)

    ident = sb("ident", [128, 128], BF16)
    negtri = sb("negtri", [128, 128], BF16)
    ones = sb("ones", [128, 128], BF16)
    negones = sb("negones", [128, 128], BF16)
    negident = sb("negident", [128, 128], BF16)
    masksb = sb("masksb", [128, 128], BF16)
    maskmb = sb("maskmb", [128, 128], BF16)
    onesf = sb("onesf", [128, 64], F32)
    gout = sb("gout", [128, 8], F32)
    gple = sb("gple", [128, 8], F32)
    epsc = sb("epsc", [128, 1], F32)
    cbuf = Buf("consts")
    P.op("dve", lambda e: e.memset(epsc[:], EPS), writes=[cbuf])
    for t, d in ((ident, ident_d), (negtri, negtri_d), (ones, ones_d), (negones, negones_d),
                 (negident, negident_d), (masksb, masksb_d), (maskmb, maskmb_d), (onesf, onesf_d), (gout, gout_d),
                 (gple, gple_d)):
        dma("sp", t[:], d, [], [cbuf])

    gz = sb("gz", [128, 8, S], BF16)
    gz_b = [[Buf("gz%d_%d" % (pr, qt)) for qt in range(NQ)] for pr in range(8)]
    ssq = sb("ssq", [128, 8, NT], F32)
    ssq_b = Buf("ssq")

    banks = [top.enter_context(nc.psum_tensor("bank%d" % i, [128, 512], F32)) for i in range(8)]
    bank_b = [Buf("bank%d" % i) for i in range(8)]

    def bank_bf(i):
        return banks[i][:].bitcast(BF16)

    stB = ExitStack()
    hT = sb("hT", [128, 8, S], BF16, stB)
    hT_b = [Buf("hT%d" % i) for i in range(NT)]

    stA = ExitStack()
    gmix = sb("gmix", [128, D], F32, stA)
    gmix_b = Buf("gmix")
    dma("sp", gmix[:], gmix_d, [], [gmix_b])
    xt = [sb("xt%d" % i, [128, D], F32, stA) for i in range(2)]
    xt_b = [Buf("xt%d" % i) for i in range(2)]
    junk = sb("junkA", [128, D], BF16, stA)
    junk_b = Buf("junkA")
    hb = [sb("hb%d" % i, [128, D], BF16, stA) for i in range(2)]
    hb_b = [Buf("hb%d" % i) for i in range(2)]
    st1 = [sb("st1_%d" % i, [128, 2], F32, stA) for i in range(2)]
    st1_b = [Buf("st1_%d" % i) for i in range(2)]
    phaseA_bufs = [gmix_b, junk_b] + xt_b + hb_b + st1_b

    def A_norm(i):
        s = i % 2
        dma("sp", xt[s][:], x_d[i * 128:(i + 1) * 128, :], [], [xt_b[s]])
        act(junk[:], xt[s][:], AF.Square, [xt_b[s]], [junk_b, st1_b[s]], accum_out=st1[s][:, 0:1])
        rstd_op(st1[s][:, 1:2], st1[s][:, 0:1], 1.0 / D, st1_b[s])
        vop("dve", "scalar_tensor_tensor", [xt_b[s], st1_b[s], gmix_b], [hb_b[s]],
            out=hb[s][:], in0=xt[s][:], scalar=st1[s][:, 1:2], in1=gmix[:],
            op0=ALU.mult, op1=ALU.mult)

    def A_tr(i):
        s = i % 2
        bk = 6 + (i % 2)
        pT = bank_bf(bk)
        for c in range(8):
            tr(pT[:, c * 128:(c + 1) * 128], hb[s][:, c * 128:(c + 1) * 128], [hb_b[s]], [bank_b[bk]])
        copy_any(i, hT[:, :, i * 128:(i + 1) * 128], pT.rearrange("p (c t) -> p c t", c=8),
                 [bank_b[bk]], [hT_b[i]])

    for i in range(NT + 1):
        if i < NT:
            A_norm(i)
        if i >= 1:
            A_tr(i - 1)
    dump("hT", hT[:], [128, 8, S], BF16, hT_b)
    stA.close()
    if stop_after == "A":
        P.emit(nc)
        return nc, dumps

    T = [sb("T%d" % i, [128, S], BF16, stB) for i in range(4)]
    T_b = [Buf("T%d" % i) for i in range(4)]
    P.alias(T_b, phaseA_bufs)
    vt = sb("vt", [128, NT, 130], BF16, stB)
    vt_b = Buf("vt")
    wbuf = [sb("wbuf0", [128, 8, 4, 128], BF16, stB)] * 2
    wbuf_b = [Buf("wbuf0")] * 2
    scr = sb("scr", [128, 3584], F32, stB)
    esb = [scr[:, 0:512], scr[:, 512:1024]]
    esb_b = [Buf("esb%d" % i) for i in range(2)]
    spb = [scr[:, 1024:1280].bitcast(BF16), scr[:, 1280:1536].bitcast(BF16)]
    spb_b = [Buf("spb%d" % i) for i in range(2)]
    rrow = [scr[:, 1536:1792].bitcast(BF16), scr[:, 1792:2048].bitcast(BF16)]
    rrow_b = [Buf("rrow%d" % i) for i in range(2)]
    sb_only = esb_b + spb_b + rrow_b
    rden = [scr[:, 0:512], scr[:, 2560:3072], scr[:, 3072:3584]]
    rden_b = [Buf("rden%d" % i) for i in range(3)]
    bcsb = scr[0:64, 512:1024]
    bcsb_b = Buf("bcsb")
    gsb = scr[:, 1024:1024 + NT * 16].rearrange("p (t n) -> p t n", n=16)
    gsb_b = Buf("gsb")
    m8 = scr[:, 1536:1536 + NT * 8].rearrange("p (t n) -> p t n", n=8)
    m8_b = Buf("m8")
    biasq = scr[:, 1792:2048].bitcast(BF16)[:, 0:NT * 16].rearrange("p (t n) -> p t n", n=16)
    biasq_b = Buf("biasq")
    biasT = scr[0:16, 2048:2560].bitcast(BF16)
    biasT_b = Buf("biasT")
    mb_only = rden_b + [bcsb_b, gsb_b, m8_b, biasq_b, biasT_b]
    ptb = [sb("ptb%d" % i, [128, 512], BF16, stB) for i in range(2)]
    ptb_b = [Buf("ptb%d" % i) for i in range(2)]
    opair = [sb("opair%d" % i, [128, 512], F32, stB) for i in range(2)]
    opair_b = [Buf("opair%d" % i) for i in range(2)]
    ostage = [sb("ostage0", [64, 512], F32, stB)] * 2
    ostage_b = [Buf("ostage0")] * 2
    osq = sb("osq", [128, 512], BF16, stB)
    osq_b = Buf("osq")
    gmask = sb("gmask", [128, NT * 16], BF16, stB)
    negcand = sb("negcand", [128, NT * 16], BF16, stB)
    fixed = sb("fixed", [128, NT * 16], BF16, stB)
    gconst_b = Buf("gconst")
    ksf = sb("ksf", [64, 16], F32, stB)
    ksb = sb("ksb", [64, 16], BF16, stB)
    ks_b = Buf("ks")
    P.alias([vt_b, gconst_b, ks_b, osq_b, wbuf_b[0], opair_b[0], opair_b[1], ostage_b[0]]
            + ptb_b + sb_only, phaseA_bufs)
    for t, d in ((gmask, gmask_d), (negcand, negcand_d), (fixed, fixed_d)):
        dma("sp", t[:], d, [], [gconst_b])
    vop("pool", "memset", [], [vt_b], ap=vt[:], constant=1.0)
    vop("dve", "memset", [], [ks_b], ap=ksb[:], constant=0.0)

    w_in_v = w_in_d.rearrange("(c p) n -> p c n", p=128)

    def load_w(pr):
        G, j = pr // 4, pr % 4
        wb = wbuf[pr % 2]
        for t in range(4):
            col0 = G * 2048 + t * 512 + j * 128
            dma("pool", wb[:, :, t, :], w_in_v[:, :, col0:col0 + 128], [], [wbuf_b[pr % 2]])

    def in_proj(pr):
        G = pr // 4
        wb = wbuf[pr % 2]
        wb_b = wbuf_b[pr % 2]
        rot = [6, 7, 0, 1, 2]
        k = [0]

        def nb():
            k[0] += 1
            return rot[k[0] % len(rot)]

        for t, scale in ((0, 0.125), (1, None)):
            for tq in range(NQ):
                cols = slice(tq * 512, (tq + 1) * 512)
                ti_ = t if G == 0 else 2 * t
                groups = [(slice(0, 128), T[ti_], T_b[ti_], 128)]
                for wc, dst, dst_b, M in groups:
                    b = nb()
                    for c in range(8):
                        mm(banks[b][0:M, :], wb[:, c, t, wc], hT[:, c, cols], c == 0,
                           [wb_b] + hT_b[tq * 4:(tq + 1) * 4], [bank_b[b]], stop=(c == 7))
                    if scale is not None:
                        if k[0] % 2 == 0:
                            act(dst[0:M, cols], banks[b][0:M, :], AF.Copy, [bank_b[b]], [dst_b],
                                scale=scale)
                        else:
                            vop("dve", "tensor_scalar", [bank_b[b]], [dst_b], out=dst[0:M, cols],
                                in0=banks[b][0:M, :], scalar1=scale, scalar2=None, op0=ALU.mult)
                    else:
                        copy_any(k[0], dst[0:M, cols], banks[b][0:M, :], [bank_b[b]], [dst_b])
        if G == 1:
            for t in range(2):
                dma("sp", T[2 * t + 1][0:64, :], T[2 * t][64:128, :], [T_b[2 * t]], [T_b[2 * t + 1]])
        for tq in range(NQ):
            cols = slice(tq * 512, (tq + 1) * 512)
            b = nb()
            for c in range(8):
                mm(banks[b][:, :], wb[:, c, 3, :], hT[:, c, cols], c == 0,
                   [wb_b] + hT_b[tq * 4:(tq + 1) * 4], [bank_b[b]], stop=(c == 7))
            act(gz[:, pr, cols], banks[b][:, :], AF.Silu, [bank_b[b]], [gz_b[pr][tq]])
        for g in range(NT // 4):
            b = nb()
            for tt in range(4):
                ti = g * 4 + tt
                for c in range(8):
                    mm(banks[b][:, tt * 128:(tt + 1) * 128], hT[:, c, ti * 128:(ti + 1) * 128],
                       wb[:, c, 2, :], c == 0, [wb_b, hT_b[ti]], [bank_b[b]], stop=(c == 7))
            bv = banks[b][:].rearrange("p (t n) -> p t n", t=4)
            P.op("act", lambda e, g=g, bv=bv: e.copy(out=vt[:, g * 4:(g + 1) * 4, 0:64],
                                                     in_=bv[:, :, 0:64]),
                 reads=[bank_b[b]], writes=[vt_b])
            P.op("dve", lambda e, g=g, bv=bv: e.tensor_copy(out=vt[:, g * 4:(g + 1) * 4, 65:129],
                                                            in_=bv[:, :, 64:128]),
                 reads=[bank_b[b]], writes=[vt_b])

    def moba_gate(h, qT, qT_b, kT, kT_b):
        dma("sp", kT[64:84, :], kaug_d[h], [], [kT_b])
        dma("sp", qT[80:84, :], qaug_d[h], [], [qT_b])
        NB = S // 256
        vop("dve", "tensor_reduce", [kT_b], [ks_b], out=ksf[:, 0:NB],
            in_=kT[0:64, :].rearrange("p (n s) -> p n s", s=256), axis=AX.X, op=ALU.add)
        vop("dve", "tensor_copy", [ks_b], [ks_b], out=ksb[:, 0:NB], in_=ksf[:, 0:NB])
        gb = 6
        for ti in range(NT):
            mm(banks[gb][:, ti * 16:(ti + 1) * 16], qT[0:64, ti * 128:(ti + 1) * 128], ksb[:, :],
               True, [qT_b, ks_b], [bank_b[gb]])
        vop("dve", "tensor_tensor", [bank_b[gb], gconst_b], [gsb_b],
            out=gsb, in0=banks[gb][:, 0:NT * 16].rearrange("p (t n) -> p t n", n=16),
            in1=gmask[:].rearrange("p (t n) -> p t n", n=16), op=ALU.add)
        for ti in range(NT):
            vop("dve", "max", [gsb_b], [m8_b], out=m8[:, ti, :], in_=gsb[:, ti, :])
        vop("dve", "tensor_tensor", [gsb_b, m8_b], [gsb_b], out=gsb, in0=gsb,
            in1=m8[:, :, 2:3].to_broadcast([128, NT, 16]), op=ALU.is_lt)
        vop("dve", "tensor_tensor", [gsb_b, gconst_b], [gsb_b], out=gsb, in0=gsb,
            in1=negcand[:].rearrange("p (t n) -> p t n", n=16), op=ALU.mult)
        vop("dve", "tensor_tensor", [gsb_b, gconst_b], [biasq_b], out=biasq, in0=gsb,
            in1=fixed[:].rearrange("p (t n) -> p t n", n=16), op=ALU.add)
        tb = 7
        pT = bank_bf(tb)
        for g in range((NT + 7) // 8):
            n = min(8, NT - g * 8)
            for tt in range(n):
                ti = g * 8 + tt
                tr(pT[0:16, tt * 128:(tt + 1) * 128], biasq[:, ti, :], [biasq_b], [bank_b[tb]])
            copy_any(g, biasT[:, 0:n * 128], pT[0:16, 0:n * 128], [bank_b[tb]], [biasT_b])
            dma("sp", qT[64:80, g * 1024:g * 1024 + n * 128], biasT[:, 0:n * 128], [biasT_b], [qT_b])

    ZB = [0, 1, 2]
    OB = [3, 4, 7]
    RB = 5
    MB = 6

    def attention(pr):
        G = pr // 4
        heads = []
        for hh in range(2):
            if G == 0:
                heads.append(dict(q=T[0], q_b=T_b[0], k=T[1], k_b=T_b[1], pb=64 * hh, K=64))
            else:
                heads.append(dict(q=T[hh], q_b=T_b[hh], k=T[2 + hh], k_b=T_b[2 + hh], pb=0, K=84))
        mask = masksb if G == 0 else maskmb
        steps = []
        gcount = 0
        for qt in (range(NQ) if G == 0 else range(NQ - 1, -1, -1)):
            for hh in range(2):
                kts = list(range(4 * qt + 3, -1, -1))
                for si, kt in enumerate(kts):
                    steps.append(dict(qt=qt, hh=hh, kt=kt, si=si, last=(si == len(kts) - 1),
                                      idx=len(steps), g=gcount))
                gcount += 1
        n = len(steps)

        def geom(st):
            j = st["kt"] - 4 * st["qt"]
            c0 = 128 * j if j > 0 else 0
            return j, c0

        def emit_Z(st):
            hd = heads[st["hh"]]
            j, c0 = geom(st)
            zb = ZB[st["idx"] % 3]
            pb, K = hd["pb"], hd["K"]
            kt, qt = st["kt"], st["qt"]
            mm(banks[zb][:, c0:512], hd["k"][pb:pb + K, kt * 128:(kt + 1) * 128],
               hd["q"][pb:pb + K, qt * 512 + c0:(qt + 1) * 512], True,
               [hd["k_b"], hd["q_b"]], [bank_b[zb]], stop=False)
            if j >= 0:
                mm(banks[zb][:, c0:c0 + 128], ident[:], mask[:], False, [cbuf], [bank_b[zb]],
                   stop=False)

        def emit_A1(st):
            j, c0 = geom(st)
            zb = ZB[st["idx"] % 3]
            s2 = st["idx"] % 2
            act(esb[s2][:, c0:512], banks[zb][:, c0:512], AF.Exp, [bank_b[zb]], [esb_b[s2]])

        def emit_A2(st):
            j, c0 = geom(st)
            s2 = st["idx"] % 2
            act(spb[s2][:, c0:512], esb[s2][:, c0:512], AF.Ln, [esb_b[s2]], [spb_b[s2]], bias=1.0)

        def emit_tri(st):
            j, c0 = geom(st)
            zb = ZB[st["idx"] % 3]
            s2 = st["idx"] % 2
            mm(banks[zb][:, c0:512], negtri[:], spb[s2][:, c0:512], False,
               [cbuf, spb_b[s2]], [bank_b[zb]], stop=False)
            if st["si"] > 0:
                cR = geom(steps[st["idx"] - 1])[1]
                mm(banks[zb][:, cR:512], negident[:], rrow[s2][:, cR:512], False,
                   [cbuf, rrow_b[s2]], [bank_b[zb]], stop=True)
            if not st["last"]:
                mm(banks[RB][:, c0:512], ones[:, :], spb[s2][:, c0:512], st["si"] == 0,
                   [cbuf, spb_b[s2]], [bank_b[RB]], stop=False)
                n2 = (st["idx"] + 1) % 2
                vop("dve", "tensor_copy", [bank_b[RB]], [rrow_b[n2]], out=rrow[n2][:, c0:512],
                    in_=banks[RB][:, c0:512])

        def emit_A3(st):
            j, c0 = geom(st)
            zb = ZB[st["idx"] % 3]
            s2 = st["idx"] % 2
            act(ptb[s2][:, c0:512], banks[zb][:, c0:512], AF.Exp, [bank_b[zb]], [ptb_b[s2]])

        def emit_PV(st):
            j, c0 = geom(st)
            s2 = st["idx"] % 2
            hh, qt, kt = st["hh"], st["qt"], st["kt"]
            ob = OB[st["g"] % 3]
            M = 64 if G == 0 else 65
            mm(banks[ob][0:M, c0:512], vt[:, kt, hh * 65:hh * 65 + M], ptb[s2][:, c0:512],
               st["si"] == 0, [vt_b, ptb_b[s2]], [bank_b[ob]], stop=st["last"])
            if st["last"]:
                if G == 0:
                    pending.append([3, "fin", st, ob])
                else:
                    pending.append([1, "ln", st, ob])

        def emit_evac(st, ob):
            hh, qt = st["hh"], st["qt"]
            op_i = qt % 2
            cols = slice(qt * 512, (qt + 1) * 512)
            if hh == 0:
                dst, dst_b = opair[op_i][0:64, :], opair_b[op_i]
            else:
                dst, dst_b = ostage[qt % 2][:, :], ostage_b[qt % 2]
            if G == 0:
                copy_any(qt + hh, dst, banks[ob][0:64, :], [bank_b[ob]], [dst_b])
            else:
                act(bcsb, banks[MB][0:64, :], AF.Exp, [bank_b[MB]], [bcsb_b], scale=-1.0)
                vop("dve", "tensor_tensor", [bank_b[ob], bcsb_b], [dst_b], out=dst,
                    in0=banks[ob][0:64, :], in1=bcsb, op=ALU.mult)
            if hh == 1:
                dma("sp", opair[op_i][64:128, :], ostage[qt % 2][:, :], [ostage_b[qt % 2]],
                    [opair_b[op_i]])
                vop("pool", "tensor_tensor", [opair_b[op_i]], [osq_b], out=osq[:, :],
                    in0=opair[op_i][:, :], in1=opair[op_i][:, :], op=ALU.mult)
                for tt in range(4):
                    mm(banks[MB][:, 508 + tt:509 + tt], osq[:, tt * 128:(tt + 1) * 128],
                       ones[:, 0:1], True, [osq_b, cbuf], [bank_b[MB]])
                vop("dve", "tensor_copy", [bank_b[MB]], [ssq_b], out=ssq[:, pr, qt * 4:(qt + 1) * 4],
                    in_=banks[MB][:, 508:512])
                vop("dve", "tensor_tensor", [opair_b[op_i], gz_b[pr][qt]], [gz_b[pr][qt]],
                    out=gz[:, pr, cols], in0=opair[op_i][:, :], in1=gz[:, pr, cols], op=ALU.mult)

        pending = []

        def run_stage(kind, st, ob):
            ri = st["g"] % 3
            if kind == "ln":
                act(rden[ri][64:65, :], banks[ob][64:65, :], AF.Ln, [bank_b[ob]], [rden_b[ri]])
                pending.append([2, "bc", st, ob])
            elif kind == "bc":
                mm(banks[MB][0:64, :], onesf[64:65, 0:64], rden[ri][64:65, :], True,
                   [cbuf, rden_b[ri]], [bank_b[MB]])
                pending.append([2, "fin", st, ob])
            else:
                emit_evac(st, ob)

        def flush(all_=False):
            while True:
                for pe_ in list(pending):
                    pe_[0] -= 1
                    if pe_[0] <= 0 or all_:
                        pending.remove(pe_)
                        run_stage(pe_[1], pe_[2], pe_[3])
                if not (all_ and pending):
                    break

        if G == 0:
            emit_Z(steps[0])
            for i in range(n + 2):
                if 0 <= i - 1 < n:
                    emit_tri(steps[i - 1])
                if 0 <= i - 2 < n:
                    emit_PV(steps[i - 2])
                if i + 1 < n:
                    emit_Z(steps[i + 1])
                if i < n:
                    emit_A1(steps[i])
                if 0 <= i - 1 < n:
                    emit_A3(steps[i - 1])
                if i < n:
                    emit_A2(steps[i])
                flush()
        else:
            emit_Z(steps[0])
            if n > 1:
                emit_Z(steps[1])
            for i in range(n + 1):
                if i < n:
                    emit_A3(steps[i])
                if 0 <= i - 1 < n:
                    emit_PV(steps[i - 1])
                if i + 2 < n:
                    emit_Z(steps[i + 2])
                flush()
        flush(True)

    npairs = 8 if stop_after is None else int(stop_after[1:]) if stop_after.startswith("B") else 8
    load_w(0)
    for pr in range(npairs):
        if pr == 4:
            P.alias(mb_only, sb_only)
        in_proj(pr)
        if pr + 1 < 8:
            load_w(pr + 1)
        if pr // 4 == 1:
            for hh in range(2):
                moba_gate((pr % 4) * 2 + hh, T[hh], T_b[hh], T[2 + hh], T_b[2 + hh])
        if "qk" in dbg and pr == int(next(iter(d for d in dbg if d.startswith("pair="))).split("=")[1]):
            for i in range(2 if pr < 4 else 4):
                dump("T%d" % i, T[i][:] if pr < 4 else T[i][0:84, :], [128 if pr < 4 else 84, S], BF16,
                     [T_b[i]])
            dump("vt", vt[:], [128, NT, 130], BF16, [vt_b])
        attention(pr)
    dump("gz", gz[:, 0:npairs, :], [128, npairs, S], BF16, [b for l in gz_b for b in l])
    dump("ssq", ssq[:, 0:npairs, :], [128, npairs, NT], F32, [ssq_b])
    phaseB_bufs = (hT_b + T_b + [vt_b, gconst_b, ks_b, osq_b, wbuf_b[0], ostage_b[0]] + ptb_b + opair_b
                   + sb_only + mb_only)
    stB.close()
    if stop_after is not None and stop_after.startswith("B"):
        P.emit(nc)
        return nc, dumps

    stC = ExitStack()
    wo = sb("wo", [128, 8, D], BF16, stC)
    wg = sb("wg", [128, 8, D], BF16, stC)
    wp = sb("wp", [128, 2, D], BF16, stC)
    wo_b, wg_b, wp_b = Buf("wo"), Buf("wg"), Buf("wp")
    wst = [sb("wst%d" % i, [128, D], F32, stC) for i in range(2)]
    wst_b = [Buf("wst%d" % i) for i in range(2)]
    gfin = sb("gfin", [128, D], F32, stC)
    gfin_b = Buf("gfin")

    def mk(name, n, shape, dt):
        return ([sb("%s%d" % (name, i), shape, dt, stC) for i in range(n)],
                [Buf("%s%d" % (name, i)) for i in range(n)])

    xc, xc_b = mk("xc", 3, [128, D], F32)
    pc, pc_b = mk("pc", 3, [128, DPLE], F32)
    pcb, pcb_b = mk("pcb", 2, [128, DPLE], BF16)
    pT_sb, pT_b = mk("pTsb", 2, [128, 2, 128], BF16)
    x1, x1_b = mk("x1_", 3, [128, D], F32)
    x1b, x1b_b = mk("x1b_", 2, [128, D], BF16)
    x1T, x1T_b = mk("x1T_", 2, [128, 8, 128], BF16)
    gate, gate_b = mk("gate", 2, [128, D], F32)
    yo, yo_b = mk("yo", 2, [128, D], F32)
    stt, stt_b = mk("stt", 3, [128, 8], F32)
    junkc = sb("junkc", [128, D], BF16, stC)
    junkc_b = Buf("junkc")
    newC = ([wo_b, wg_b, wp_b, gfin_b, junkc_b] + wst_b + xc_b + pc_b + pcb_b + pT_b + x1_b + x1b_b
            + x1T_b + gate_b + yo_b + stt_b)
    P.alias(newC, phaseB_bufs)

    dma("sp", gfin[:], gfin_d, [], [gfin_b])
    w_out_v = w_out_d.rearrange("(c p) n -> p c n", p=128)
    w_gate_v = w_gate_d.rearrange("(c p) n -> p c n", p=128)
    k = 0
    for (wv, wt, wt_b, gv) in ((w_out_v, wo, wo_b, gout), (w_gate_v, wg, wg_b, gple)):
        for c in range(8):
            s = k % 2
            dma("sp", wst[s][:], wv[:, c, :], [], [wst_b[s]])
            eng = "dve" if k % 2 == 0 else "act"
            if eng == "dve":
                vop("dve", "tensor_scalar", [wst_b[s], cbuf], [wt_b], out=wt[:, c, :], in0=wst[s][:],
                    scalar1=gv[:, c:c + 1], scalar2=None, op0=ALU.mult)
            else:
                act(wt[:, c, :], wst[s][:], AF.Copy, [wst_b[s], cbuf], [wt_b], scale=gv[:, c:c + 1])
            k += 1
    dma("pool", wp[:], w_ple_d.rearrange("(c p) n -> p c n", p=128), [], [wp_b])

    rg = sb("rg", [128, NT, 2], F32, stC)
    rg_b = Buf("rg")
    P.alias([rg_b], phaseB_bufs)
    vop("dve", "tensor_reduce", [ssq_b], [rg_b], out=rg[:],
        in_=ssq[:].rearrange("p (g c) t -> p t g c", g=2), axis=AX.X, op=ALU.add)
    rstd_op(rg[:].rearrange("p t g -> p (t g)"), rg[:].rearrange("p t g -> p (t g)"), 1.0 / 512, rg_b)

    def LOAD(i):
        s3 = i % 3
        tok = slice(i * 128, (i + 1) * 128)
        dma("sp", xc[s3][:], x_d[tok, :], [], [xc_b[s3]])
        dma("sp", pc[s3][:], p_d[tok, :], [], [pc_b[s3]])

    def OPX(i):
        s2, s3 = i % 2, i % 3
        tok = slice(i * 128, (i + 1) * 128)
        for g in range(2):
            for hf in range(2):
                b = g * 2 + hf
                for c4 in range(4):
                    c = g * 4 + c4
                    mm(banks[b][:, :], gz[:, c, tok], wo[:, c, hf * 512:(hf + 1) * 512], c4 == 0,
                       gz_b[c] + [wo_b], [bank_b[b]], stop=(c4 == 3))
        for hf in range(2):
            cs = slice(hf * 512, (hf + 1) * 512)
            vop("dve", "scalar_tensor_tensor", [bank_b[hf], rg_b, xc_b[s3]], [x1_b[s3]],
                out=x1[s3][:, cs], in0=banks[hf][:, :], scalar=rg[:, i, 0:1], in1=xc[s3][:, cs],
                op0=ALU.mult, op1=ALU.add)
            vop("dve", "scalar_tensor_tensor", [bank_b[2 + hf], rg_b, x1_b[s3]], [x1_b[s3]],
                out=x1[s3][:, cs], in0=banks[2 + hf][:, :], scalar=rg[:, i, 1:2], in1=x1[s3][:, cs],
                op0=ALU.mult, op1=ALU.add)
        vop("pool", "tensor_copy", [pc_b[s3]], [pcb_b[s2]], out=pcb[s2][:], in_=pc[s3][:])
        pT5 = bank_bf(5)
        for c in range(2):
            tr(pT5[:, c * 128:(c + 1) * 128], pcb[s2][:, c * 128:(c + 1) * 128], [pcb_b[s2]], [bank_b[5]])
        copy_any(1, pT_sb[s2][:], pT5[:, 0:256].rearrange("p (c t) -> p c t", c=2), [bank_b[5]],
                 [pT_b[s2]])

    def CST(i):
        s2, s3 = i % 2, i % 3
        P.op("act", lambda e: e.copy(out=x1b[s2][:], in_=x1[s3][:]), reads=[x1_b[s3]],
             writes=[x1b_b[s2]])
        act(junkc[:], x1[s3][:], AF.Square, [x1_b[s3]], [junkc_b, stt_b[s3]],
            accum_out=stt[s3][:, 2:3])
        rstd_op(stt[s3][:, 3:4], stt[s3][:, 2:3], 1.0 / D, stt_b[s3])

    def TRE(i):
        s2, s3 = i % 2, i % 3
        pT = bank_bf(4)
        for c in range(8):
            tr(pT[:, c * 128:(c + 1) * 128], x1b[s2][:, c * 128:(c + 1) * 128], [x1b_b[s2]], [bank_b[4]])
        copy_any(0, x1T[s2][:], pT.rearrange("p (c t) -> p c t", c=8), [bank_b[4]], [x1T_b[s2]])

    def S2(i):
        s2, s3 = i % 2, i % 3
        for hf in range(2):
            cs = slice(hf * 512, (hf + 1) * 512)
            gb = 6 if hf == 0 else 5
            for c in range(8):
                mm(banks[gb][:, :], x1T[s2][:, c, :], wg[:, c, cs], c == 0, [x1T_b[s2], wg_b],
                   [bank_b[gb]], stop=(c == 7))
            act(gate[s2][:, cs], banks[gb][:, :], AF.Sigmoid, [bank_b[gb], stt_b[s3]], [gate_b[s2]],
                scale=stt[s3][:, 3:4])
            for c in range(2):
                mm(banks[7][:, :], pT_sb[s2][:, c, :], wp[:, c, cs], c == 0, [pT_b[s2], wp_b],
                   [bank_b[7]], stop=(c == 1))
            vop("dve", "tensor_tensor", [bank_b[7], gate_b[s2]], [gate_b[s2]], out=gate[s2][:, cs],
                in0=banks[7][:, :], in1=gate[s2][:, cs], op=ALU.mult)
            vop("pool", "tensor_tensor", [gate_b[s2], x1_b[s3]], [x1_b[s3]], out=x1[s3][:, cs],
                in0=gate[s2][:, cs], in1=x1[s3][:, cs], op=ALU.add)

    def S3a(i):
        s3 = i % 3
        act(junkc[:], x1[s3][:], AF.Square, [x1_b[s3]], [junkc_b, stt_b[s3]],
            accum_out=stt[s3][:, 4:5])

    def S3b(i):
        s2, s3 = i % 2, i % 3
        tok = slice(i * 128, (i + 1) * 128)
        rstd_op(stt[s3][:, 5:6], stt[s3][:, 4:5], 1.0 / D, stt_b[s3])
        vop("dve", "scalar_tensor_tensor", [x1_b[s3], stt_b[s3], gfin_b], [yo_b[s2]], out=yo[s2][:],
            in0=x1[s3][:], scalar=stt[s3][:, 5:6], in1=gfin[:], op0=ALU.mult, op1=ALU.mult)
        dma("sp", out_d[tok, :], yo[s2][:], [yo_b[s2]], [])

    LOAD(0)
    if NT > 1:
        LOAD(1)
    for t in range(-1, NT + 1):
        if 0 <= t + 3 < NT:
            LOAD(t + 3)
        if 0 <= t < NT:
            TRE(t)
        if 0 <= t + 1 < NT:
            OPX(t + 1)
        if 0 <= t - 1 < NT:
            S3a(t - 1)
        if 0 <= t < NT:
            S2(t)
        if 0 <= t + 1 < NT:
            CST(t + 1)
        if 0 <= t - 1 < NT:
            S3b(t - 1)
    stC.close()
    top.close()
    P.emit(nc)
    return nc, dumps


def _host_inputs(S, x, p, w_in, g_mix, g_out_sb, g_out_mb, w_out, w_ple, g_ple, w_ple_gate, g_final):
    c = _consts(S)
    f = np.float32
    shared = {
        "w_in": np.ascontiguousarray(w_in[0], f),
        "w_out": np.ascontiguousarray(w_out[0], f),
        "w_ple": np.ascontiguousarray(w_ple[0], f),
        "w_gate": np.ascontiguousarray(w_ple_gate[0], f),
        "gmix_bc": np.ascontiguousarray(np.broadcast_to(np.asarray(g_mix[0], f)[None, :], (128, D))),
        "gfin_bc": np.ascontiguousarray(np.broadcast_to(np.asarray(g_final, f)[None, :], (128, D))),
        "gout_pc": np.ascontiguousarray(
            np.concatenate([np.asarray(g_out_sb[0], f), np.asarray(g_out_mb[0], f)]).reshape(8, 128).T),
        "gple_pc": np.ascontiguousarray(np.asarray(g_ple[0], f).reshape(8, 128).T),
    }
    shared.update(c)
    maps = []
    for b in range(x.shape[0]):
        m = dict(shared)
        m["x"] = np.ascontiguousarray(x[b], f)
        m["p"] = np.ascontiguousarray(p[0, b], f)
        maps.append(m)
    return maps


_CACHE = {}


def kernel(x, p, w_in, g_mix, g_out_sb, g_out_mb, w_out, w_ple, g_ple, w_ple_gate, g_final):
    x = np.asarray(x)
    B, S, _ = x.shape
    if S not in _CACHE:
        _CACHE[S] = build(S)[0]
    nc = _CACHE[S]
    maps = _host_inputs(S, x, np.asarray(p), np.asarray(w_in), np.asarray(g_mix), np.asarray(g_out_sb),
                        np.asarray(g_out_mb), np.asarray(w_out), np.asarray(w_ple), np.asarray(g_ple),
                        np.asarray(w_ple_gate), np.asarray(g_final))
    res = run_bass_kernel_spmd(nc, maps, core_ids=list(range(B)))
    return np.stack([np.asarray(r["out"], np.float32) for r in res.results], axis=0)
```

```python
from contextlib import ExitStack

import numpy as np
import ml_dtypes
import concourse.bass as bass
import concourse.mybir as mybir
from concourse.bass_utils import run_bass_kernel_spmd

F32 = mybir.dt.float32
BF16 = mybir.dt.bfloat16
AF = mybir.ActivationFunctionType
ALU = mybir.AluOpType
AX = mybir.AxisListType

D = 1024
DPLE = 256
EPS = 1e-6
NEG = -30000.0
ENGS = ("pe", "act", "dve", "pool", "sp")


class Buf:
    __slots__ = ("name", "w", "r")

    def __init__(self, name=""):
        self.name = name
        self.w = None
        self.r = {}


class Prog:
    def __init__(self, n_dma_sp=12, n_dma_pool=6):
        self.ops = {e: [] for e in ENGS}
        self.cnt = {e: 0 for e in ENGS}
        self.seen = {e: {} for e in ENGS}
        self.dma_pool = {"sp": [("dma", "sp", i) for i in range(n_dma_sp)],
                         "pool": [("dma", "pool", i) for i in range(n_dma_pool)]}
        self.dma_rr = {"sp": 0, "pool": 0}
        self.dma_cnt = {}
        for q in self.dma_pool.values():
            for k in q:
                self.dma_cnt[k] = 0

    def op(self, eng, fn, reads=(), writes=(), dma=False):
        deps = {}

        def add(ev, kind):
            if ev is None:
                return
            key, val = ev
            if key == eng and eng == "pe":
                return
            if deps.get(key, 0) < val:
                deps[key] = val

        for b in reads:
            add(b.w, "raw")
        for b in writes:
            add(b.w, "waw")
            for key, val in b.r.items():
                add((key, val), "war")
        if dma:
            pool = self.dma_pool[eng]
            k = pool[self.dma_rr[eng] % len(pool)]
            self.dma_rr[eng] += 1
            prev = self.dma_cnt[k]
            if prev > 0 and deps.get(k, 0) < prev:
                deps[k] = prev
            self.dma_cnt[k] = prev + 16
            ev = (k, prev + 16)
        else:
            self.cnt[eng] += 1
            ev = (eng, self.cnt[eng])
        seen = self.seen[eng]
        waits = []
        for key, val in deps.items():
            if seen.get(key, 0) < val:
                waits.append((key, val))
                seen[key] = val
        self.ops[eng].append((fn, waits, ev, dma))
        for b in reads:
            if b.r.get(ev[0], 0) < ev[1]:
                b.r[ev[0]] = ev[1]
        for b in writes:
            b.w = ev
            b.r = {}
        return ev

    def alias(self, new_bufs, old_bufs):
        merged = {}
        for b in old_bufs:
            evs = list(b.r.items())
            if b.w is not None:
                evs.append(b.w)
            for key, val in evs:
                if merged.get(key, 0) < val:
                    merged[key] = val
        for b in new_bufs:
            for key, val in merged.items():
                if b.r.get(key, 0) < val:
                    b.r[key] = val

    def emit(self, nc):
        sem_keys = list(ENGS[:4]) + [k for q in self.dma_pool.values() for k in q]
        with ExitStack() as st:
            sems = {}
            for k in sem_keys:
                nm = k if isinstance(k, str) else "d_%s_%d" % (k[1], k[2])
                sems[k] = st.enter_context(nc.semaphore("s_" + nm))
            final_waits = [(k, v) for k, v in self.dma_cnt.items() if v > 0]
            block = st.enter_context(nc.Block())

            def replay(name, e):
                for fn, waits, ev, dma in self.ops[name]:
                    for key, val in waits[:-1]:
                        e.wait_ge(sems[key], val)
                    ins = fn(e)
                    if waits:
                        ins._wait_ge(sems[waits[-1][0]], waits[-1][1])
                    ins.then_inc(sems[ev[0]], 16 if dma else 1)
                if name == "sp":
                    for key, val in final_waits:
                        e.wait_ge(sems[key], val)

            @block.tensor
            def _(e):
                replay("pe", e)

            @block.scalar
            def _(e):
                replay("act", e)

            @block.vector
            def _(e):
                replay("dve", e)

            @block.gpsimd
            def _(e):
                replay("pool", e)

            @block.sync
            def _(e):
                replay("sp", e)


def _consts(S):
    bf = ml_dtypes.bfloat16
    NT = S // 128
    i = np.arange(128)
    c = {}
    c["ident"] = np.eye(128, dtype=np.float32).astype(bf)
    c["negtri"] = (-(i[:, None] >= i[None, :]).astype(np.float32)).astype(bf)
    c["negident"] = (-np.eye(128, dtype=np.float32)).astype(bf)
    c["ones"] = np.ones((128, 128), np.float32).astype(bf)
    c["negones"] = (-np.ones((128, 128), np.float32)).astype(bf)
    c["masksb"] = np.where(i[:, None] < i[None, :], 0.0, NEG).astype(np.float32).astype(bf)
    c["maskmb"] = np.where(i[:, None] <= i[None, :], 0.0, NEG).astype(np.float32).astype(bf)
    c["onesf"] = np.ones((128, 64), np.float32)
    pos = np.arange(S)
    hi = (pos // 256) * 256.0
    lo = (pos % 256) * 1.0
    kaug = np.zeros((8, 20, S), np.float32)
    qaug = np.zeros((8, 4, S), np.float32)
    for h in range(8):
        sl = 2.0 ** (-8.0 * (h + 1) / 8.0)
        for n in range(16):
            kaug[h, n] = (pos // 256 == n)
        kaug[h, 16] = 1.0
        kaug[h, 17] = 1.0
        kaug[h, 18] = sl * hi
        kaug[h, 19] = sl * lo
        qaug[h, 0] = -sl * hi
        qaug[h, 1] = -sl * lo
        qaug[h, 2] = 1.0
        qaug[h, 3] = 1.0
    c["kaug"] = kaug.astype(bf)
    c["qaug"] = qaug.astype(bf)
    gmask = np.zeros((NT, 16), np.float32)
    negcand = np.zeros((NT, 16), np.float32)
    fixed = np.zeros((NT, 16), np.float32)
    for ti in range(NT):
        qb = ti // 2
        for n in range(16):
            if n < qb:
                negcand[ti, n] = NEG
            else:
                gmask[ti, n] = -1e30
            if n > qb:
                fixed[ti, n] = NEG
    c["gmask"] = np.broadcast_to(gmask.reshape(1, NT * 16), (128, NT * 16)).astype(bf)
    c["negcand"] = np.broadcast_to(negcand.reshape(1, NT * 16), (128, NT * 16)).astype(bf)
    c["fixed"] = np.broadcast_to(fixed.reshape(1, NT * 16), (128, NT * 16)).astype(bf)
    return c


def build(S, dbg=None, stop_after=None):
    dbg = set(dbg or ())
    if "qk" in dbg:
        dbg |= {"T0", "T1", "T2", "T3", "vt"}
    NT = S // 128
    NQ = S // 512
    nc = bass.Bass("TRN2", target_bir_lowering=False)
    P = Prog()

    def din(name, shape, dt=F32):
        return nc.dram_tensor(name, list(shape), dt, kind="ExternalInput").ap()

    x_d = din("x", [S, D])
    p_d = din("p", [S, DPLE])
    w_in_d = din("w_in", [D, 4 * D])
    w_out_d = din("w_out", [D, D])
    w_ple_d = din("w_ple", [DPLE, D])
    w_gate_d = din("w_gate", [D, D])
    gmix_d = din("gmix_bc", [128, D])
    gfin_d = din("gfin_bc", [128, D])
    gout_d = din("gout_pc", [128, 8])
    gple_d = din("gple_pc", [128, 8])
    ident_d = din("ident", [128, 128], BF16)
    negtri_d = din("negtri", [128, 128], BF16)
    ones_d = din("ones", [128, 128], BF16)
    negones_d = din("negones", [128, 128], BF16)
    negident_d = din("negident", [128, 128], BF16)
    masksb_d = din("masksb", [128, 128], BF16)
    maskmb_d = din("maskmb", [128, 128], BF16)
    onesf_d = din("onesf", [128, 64])
    kaug_d = din("kaug", [8, 20, S], BF16)
    qaug_d = din("qaug", [8, 4, S], BF16)
    gmask_d = din("gmask", [128, NT * 16], BF16)
    negcand_d = din("negcand", [128, NT * 16], BF16)
    fixed_d = din("fixed", [128, NT * 16], BF16)
    out_d = nc.dram_tensor("out", [S, D], F32, kind="ExternalOutput").ap()

    dumps = {}
    top = ExitStack()

    def sb(name, shape, dt, st=top):
        return st.enter_context(nc.sbuf_tensor("sb_" + name, list(shape), dt))

    def dump(name, src_ap, shape, dt, reads):
        if name not in dbg:
            return
        t = nc.dram_tensor("dbg_" + name, list(shape), dt, kind="ExternalOutput").ap()
        dumps[name] = t
        P.op("sp", lambda e, t=t, s=src_ap: e.dma_start(out=t, in_=s), reads=reads, dma=True)

    def mm(out, lhsT, rhs, start, reads, writes, stop=True):
        P.op("pe", lambda e: e.matmul(out, lhsT, rhs, start=start, stop=stop,
                                      skip_group_check=True), reads=reads, writes=writes)

    def tr(out, in_, reads, writes):
        P.op("pe", lambda e: e.transpose(out=out, in_=in_, identity=ident[:]),
             reads=list(reads) + [cbuf], writes=writes)

    def act(out, in_, func, reads, writes, **kw):
        P.op("act", lambda e: e.activation(out=out, in_=in_, func=func, **kw),
             reads=reads, writes=writes)

    def dma(eng, out, in_, reads, writes):
        P.op(eng, lambda e: e.dma_start(out=out, in_=in_), reads=reads, writes=writes, dma=True)

    def vop(eng, name, reads, writes, **kw):
        P.op(eng, lambda e: getattr(e, name)(**kw), reads=reads, writes=writes)

    def rstd_op(dst, src, scale, b):
        act(dst, src, AF.Sqrt, [b, cbuf], [b], scale=scale, bias=epsc[0:dst.shape[0], 0:1])
        vop("dve", "reciprocal", [b], [b], out=dst, in_=dst)

    def copy_any(k, out, in_, reads, writes):
        if k % 2 == 0:
            P.op("act", lambda e: e.copy(out=out, in_=in_), reads=reads, writes=writes)
        else:
            P.op("dve", lambda e: e.tensor_copy(out=out, in_=in_), reads=reads, writes=writes)

    ident = sb("ident", [128, 128], BF16)
    negtri = sb("negtri", [128, 128], BF16)
    ones = sb("ones", [128, 128], BF16)
    negones = sb("negones", [128, 128], BF16)
    negident = sb("negident", [128, 128], BF16)
    masksb = sb("masksb", [128, 128], BF16)
    maskmb = sb("maskmb", [128, 128], BF16)
    onesf = sb("onesf", [128, 64], F32)
    gout = sb("gout", [128, 8], F32)
    gple = sb("gple", [128, 8], F32)
    epsc = sb("epsc", [128, 1], F32)
    cbuf = Buf("consts")
    P.op("dve", lambda e: e.memset(epsc[:], EPS), writes=[cbuf])
    for t, d in ((ident, ident_d), (negtri, negtri_d), (ones, ones_d), (negones, negones_d),
                 (negident, negident_d), (masksb, masksb_d), (maskmb, maskmb_d), (onesf, onesf_d), (gout, gout_d),
                 (gple, gple_d)):
        dma("sp", t[:], d, [], [cbuf])

    gz = sb("gz", [128, 8, S], BF16)
    gz_b = [[Buf("gz%d_%d" % (pr, qt)) for qt in range(NQ)] for pr in range(8)]
    ssq = sb("ssq", [128, 8, NT], F32)
    ssq_b = Buf("ssq")

    banks = [top.enter_context(nc.psum_tensor("bank%d" % i, [128, 512], F32)) for i in range(8)]
    bank_b = [Buf("bank%d" % i) for i in range(8)]

    def bank_bf(i):
        return banks[i][:].bitcast(BF16)

    stB = ExitStack()
    hT = sb("hT", [128, 8, S], BF16, stB)
    hT_b = [Buf("hT%d" % i) for i in range(NT)]

    stA = ExitStack()
    gmix = sb("gmix", [128, D], F32, stA)
    gmix_b = Buf("gmix")
    dma("sp", gmix[:], gmix_d, [], [gmix_b])
    xt = [sb("xt%d" % i, [128, D], F32, stA) for i in range(2)]
    xt_b = [Buf("xt%d" % i) for i in range(2)]
    junk = sb("junkA", [128, D], BF16, stA)
    junk_b = Buf("junkA")
    hb = [sb("hb%d" % i, [128, D], BF16, stA) for i in range(2)]
    hb_b = [Buf("hb%d" % i) for i in range(2)]
    st1 = [sb("st1_%d" % i, [128, 2], F32, stA) for i in range(2)]
    st1_b = [Buf("st1_%d" % i) for i in range(2)]
    phaseA_bufs = [gmix_b, junk_b] + xt_b + hb_b + st1_b

    def A_norm(i):
        s = i % 2
        dma("sp", xt[s][:], x_d[i * 128:(i + 1) * 128, :], [], [xt_b[s]])
        act(junk[:], xt[s][:], AF.Square, [xt_b[s]], [junk_b, st1_b[s]], accum_out=st1[s][:, 0:1])
        rstd_op(st1[s][:, 1:2], st1[s][:, 0:1], 1.0 / D, st1_b[s])
        vop("dve", "scalar_tensor_tensor", [xt_b[s], st1_b[s], gmix_b], [hb_b[s]],
            out=hb[s][:], in0=xt[s][:], scalar=st1[s][:, 1:2], in1=gmix[:],
            op0=ALU.mult, op1=ALU.mult)

    def A_tr(i):
        s = i % 2
        bk = 6 + (i % 2)
        pT = bank_bf(bk)
        for c in range(8):
            tr(pT[:, c * 128:(c + 1) * 128], hb[s][:, c * 128:(c + 1) * 128], [hb_b[s]], [bank_b[bk]])
        copy_any(i, hT[:, :, i * 128:(i + 1) * 128], pT.rearrange("p (c t) -> p c t", c=8),
                 [bank_b[bk]], [hT_b[i]])

    for i in range(NT + 1):
        if i < NT:
            A_norm(i)
        if i >= 1:
            A_tr(i - 1)
    dump("hT", hT[:], [128, 8, S], BF16, hT_b)
    stA.close()
    if stop_after == "A":
        P.emit(nc)
        return nc, dumps

    T = [sb("T%d" % i, [128, S], BF16, stB) for i in range(4)]
    T_b = [Buf("T%d" % i) for i in range(4)]
    P.alias(T_b, phaseA_bufs)
    vt = sb("vt", [128, NT, 130], BF16, stB)
    vt_b = Buf("vt")
    wbuf = [sb("wbuf0", [128, 8, 4, 128], BF16, stB)] * 2
    wbuf_b = [Buf("wbuf0")] * 2
    scr = sb("scr", [128, 3584], F32, stB)
    esb = [scr[:, 0:512], scr[:, 512:1024]]
    esb_b = [Buf("esb%d" % i) for i in range(2)]
    spb = [scr[:, 1024:1280].bitcast(BF16), scr[:, 1280:1536].bitcast(BF16)]
    spb_b = [Buf("spb%d" % i) for i in range(2)]
    rrow = [scr[:, 1536:1792].bitcast(BF16), scr[:, 1792:2048].bitcast(BF16)]
    rrow_b = [Buf("rrow%d" % i) for i in range(2)]
    sb_only = esb_b + spb_b + rrow_b
    rden = [scr[:, 0:512], scr[:, 2560:3072], scr[:, 3072:3584]]
    rden_b = [Buf("rden%d" % i) for i in range(3)]
    bcsb = scr[0:64, 512:1024]
    bcsb_b = Buf("bcsb")
    gsb = scr[:, 1024:1024 + NT * 16].rearrange("p (t n) -> p t n", n=16)
    gsb_b = Buf("gsb")
    m8 = scr[:, 1536:1536 + NT * 8].rearrange("p (t n) -> p t n", n=8)
    m8_b = Buf("m8")
    biasq = scr[:, 1792:2048].bitcast(BF16)[:, 0:NT * 16].rearrange("p (t n) -> p t n", n=16)
    biasq_b = Buf("biasq")
    biasT = scr[0:16, 2048:2560].bitcast(BF16)
    biasT_b = Buf("biasT")
    mb_only = rden_b + [bcsb_b, gsb_b, m8_b, biasq_b, biasT_b]
    ptb = [sb("ptb%d" % i, [128, 512], BF16, stB) for i in range(2)]
    ptb_b = [Buf("ptb%d" % i) for i in range(2)]
    opair = [sb("opair%d" % i, [128, 512], F32, stB) for i in range(2)]
    opair_b = [Buf("opair%d" % i) for i in range(2)]
    ostage = [sb("ostage0", [64, 512], F32, stB)] * 2
    ostage_b = [Buf("ostage0")] * 2
    osq = sb("osq", [128, 512], BF16, stB)
    osq_b = Buf("osq")
    gmask = sb("gmask", [128, NT * 16], BF16, stB)
    negcand = sb("negcand", [128, NT * 16], BF16, stB)
    fixed = sb("fixed", [128, NT * 16], BF16, stB)
    gconst_b = Buf("gconst")
    ksf = sb("ksf", [64, 16], F32, stB)
    ksb = sb("ksb", [64, 16], BF16, stB)
    ks_b = Buf("ks")
    P.alias([vt_b, gconst_b, ks_b, osq_b, wbuf_b[0], opair_b[0], opair_b[1], ostage_b[0]]
            + ptb_b + sb_only, phaseA_bufs)
    for t, d in ((gmask, gmask_d), (negcand, negcand_d), (fixed, fixed_d)):
        dma("sp", t[:], d, [], [gconst_b])
    vop("pool", "memset", [], [vt_b], ap=vt[:], constant=1.0)
    vop("dve", "memset", [], [ks_b], ap=ksb[:], constant=0.0)

    w_in_v = w_in_d.rearrange("(c p) n -> p c n", p=128)

    def load_w(pr):
        G, j = pr // 4, pr % 4
        wb = wbuf[pr % 2]
        for t in range(4):
            col0 = G * 2048 + t * 512 + j * 128
            dma("pool", wb[:, :, t, :], w_in_v[:, :, col0:col0 + 128], [], [wbuf_b[pr % 2]])

    def in_proj(pr):
        G = pr // 4
        wb = wbuf[pr % 2]
        wb_b = wbuf_b[pr % 2]
        rot = [6, 7, 0, 1, 2]
        k = [0]

        def nb():
            k[0] += 1
            return rot[k[0] % len(rot)]

        for t, scale in ((0, 0.125), (1, None)):
            for tq in range(NQ):
                cols = slice(tq * 512, (tq + 1) * 512)
                ti_ = t if G == 0 else 2 * t
                groups = [(slice(0, 128), T[ti_], T_b[ti_], 128)]
                for wc, dst, dst_b, M in groups:
                    b = nb()
                    for c in range(8):
                        mm(banks[b][0:M, :], wb[:, c, t, wc], hT[:, c, cols], c == 0,
                           [wb_b] + hT_b[tq * 4:(tq + 1) * 4], [bank_b[b]], stop=(c == 7))
                    if scale is not None:
                        if k[0] % 2 == 0:
                            act(dst[0:M, cols], banks[b][0:M, :], AF.Copy, [bank_b[b]], [dst_b],
                                scale=scale)
                        else:
                            vop("dve", "tensor_scalar", [bank_b[b]], [dst_b], out=dst[0:M, cols],
                                in0=banks[b][0:M, :], scalar1=scale, scalar2=None, op0=ALU.mult)
                    else:
                        copy_any(k[0], dst[0:M, cols], banks[b][0:M, :], [bank_b[b]], [dst_b])
        if G == 1:
            for t in range(2):
                dma("sp", T[2 * t + 1][0:64, :], T[2 * t][64:128, :], [T_b[2 * t]], [T_b[2 * t + 1]])
        for tq in range(NQ):
            cols = slice(tq * 512, (tq + 1) * 512)
            b = nb()
            for c in range(8):
                mm(banks[b][:, :], wb[:, c, 3, :], hT[:, c, cols], c == 0,
                   [wb_b] + hT_b[tq * 4:(tq + 1) * 4], [bank_b[b]], stop=(c == 7))
            act(gz[:, pr, cols], banks[b][:, :], AF.Silu, [bank_b[b]], [gz_b[pr][tq]])
        for g in range(NT // 4):
            b = nb()
            for tt in range(4):
                ti = g * 4 + tt
                for c in range(8):
                    mm(banks[b][:, tt * 128:(tt + 1) * 128], hT[:, c, ti * 128:(ti + 1) * 128],
                       wb[:, c, 2, :], c == 0, [wb_b, hT_b[ti]], [bank_b[b]], stop=(c == 7))
            bv = banks[b][:].rearrange("p (t n) -> p t n", t=4)
            P.op("act", lambda e, g=g, bv=bv: e.copy(out=vt[:, g * 4:(g + 1) * 4, 0:64],
                                                     in_=bv[:, :, 0:64]),
                 reads=[bank_b[b]], writes=[vt_b])
            P.op("dve", lambda e, g=g, bv=bv: e.tensor_copy(out=vt[:, g * 4:(g + 1) * 4, 65:129],
                                                            in_=bv[:, :, 64:128]),
                 reads=[bank_b[b]], writes=[vt_b])

    def moba_gate(h, qT, qT_b, kT, kT_b):
        dma("sp", kT[64:84, :], kaug_d[h], [], [kT_b])
        dma("sp", qT[80:84, :], qaug_d[h], [], [qT_b])
        NB = S // 256
        vop("dve", "tensor_reduce", [kT_b], [ks_b], out=ksf[:, 0:NB],
            in_=kT[0:64, :].rearrange("p (n s) -> p n s", s=256), axis=AX.X, op=ALU.add)
        vop("dve", "tensor_copy", [ks_b], [ks_b], out=ksb[:, 0:NB], in_=ksf[:, 0:NB])
        gb = 6
        for ti in range(NT):
            mm(banks[gb][:, ti * 16:(ti + 1) * 16], qT[0:64, ti * 128:(ti + 1) * 128], ksb[:, :],
               True, [qT_b, ks_b], [bank_b[gb]])
        vop("dve", "tensor_tensor", [bank_b[gb], gconst_b], [gsb_b],
            out=gsb, in0=banks[gb][:, 0:NT * 16].rearrange("p (t n) -> p t n", n=16),
            in1=gmask[:].rearrange("p (t n) -> p t n", n=16), op=ALU.add)
        for ti in range(NT):
            vop("dve", "max", [gsb_b], [m8_b], out=m8[:, ti, :], in_=gsb[:, ti, :])
        vop("dve", "tensor_tensor", [gsb_b, m8_b], [gsb_b], out=gsb, in0=gsb,
            in1=m8[:, :, 2:3].to_broadcast([128, NT, 16]), op=ALU.is_lt)
        vop("dve", "tensor_tensor", [gsb_b, gconst_b], [gsb_b], out=gsb, in0=gsb,
            in1=negcand[:].rearrange("p (t n) -> p t n", n=16), op=ALU.mult)
        vop("dve", "tensor_tensor", [gsb_b, gconst_b], [biasq_b], out=biasq, in0=gsb,
            in1=fixed[:].rearrange("p (t n) -> p t n", n=16), op=ALU.add)
        tb = 7
        pT = bank_bf(tb)
        for g in range((NT + 7) // 8):
            n = min(8, NT - g * 8)
            for tt in range(n):
                ti = g * 8 + tt
                tr(pT[0:16, tt * 128:(tt + 1) * 128], biasq[:, ti, :], [biasq_b], [bank_b[tb]])
            copy_any(g, biasT[:, 0:n * 128], pT[0:16, 0:n * 128], [bank_b[tb]], [biasT_b])
            dma("sp", qT[64:80, g * 1024:g * 1024 + n * 128], biasT[:, 0:n * 128], [biasT_b], [qT_b])

    ZB = [0, 1, 2]
    OB = [3, 4, 7]
    RB = 5
    MB = 6

    def attention(pr):
        G = pr // 4
        heads = []
        for hh in range(2):
            if G == 0:
                heads.append(dict(q=T[0], q_b=T_b[0], k=T[1], k_b=T_b[1], pb=64 * hh, K=64))
            else:
                heads.append(dict(q=T[hh], q_b=T_b[hh], k=T[2 + hh], k_b=T_b[2 + hh], pb=0, K=84))
        mask = masksb if G == 0 else maskmb
        steps = []
        gcount = 0
        for qt in (range(NQ) if G == 0 else range(NQ - 1, -1, -1)):
            for hh in range(2):
                kts = list(range(4 * qt + 3, -1, -1))
                for si, kt in enumerate(kts):
                    steps.append(dict(qt=qt, hh=hh, kt=kt, si=si, last=(si == len(kts) - 1),
                                      idx=len(steps), g=gcount))
                gcount += 1
        n = len(steps)

        def geom(st):
            j = st["kt"] - 4 * st["qt"]
            c0 = 128 * j if j > 0 else 0
            return j, c0

        def emit_Z(st):
            hd = heads[st["hh"]]
            j, c0 = geom(st)
            zb = ZB[st["idx"] % 3]
            pb, K = hd["pb"], hd["K"]
            kt, qt = st["kt"], st["qt"]
            mm(banks[zb][:, c0:512], hd["k"][pb:pb + K, kt * 128:(kt + 1) * 128],
               hd["q"][pb:pb + K, qt * 512 + c0:(qt + 1) * 512], True,
               [hd["k_b"], hd["q_b"]], [bank_b[zb]], stop=False)
            if j >= 0:
                mm(banks[zb][:, c0:c0 + 128], ident[:], mask[:], False, [cbuf], [bank_b[zb]],
                   stop=False)

        def emit_A1(st):
            j, c0 = geom(st)
            zb = ZB[st["idx"] % 3]
            s2 = st["idx"] % 2
            act(esb[s2][:, c0:512], banks[zb][:, c0:512], AF.Exp, [bank_b[zb]], [esb_b[s2]])

        def emit_A2(st):
            j, c0 = geom(st)
            s2 = st["idx"] % 2
            act(spb[s2][:, c0:512], esb[s2][:, c0:512], AF.Ln, [esb_b[s2]], [spb_b[s2]], bias=1.0)

        def emit_tri(st):
            j, c0 = geom(st)
            zb = ZB[st["idx"] % 3]
            s2 = st["idx"] % 2
            mm(banks[zb][:, c0:512], negtri[:], spb[s2][:, c0:512], False,
               [cbuf, spb_b[s2]], [bank_b[zb]], stop=False)
            if st["si"] > 0:
                cR = geom(steps[st["idx"] - 1])[1]
                mm(banks[zb][:, cR:512], negident[:], rrow[s2][:, cR:512], False,
                   [cbuf, rrow_b[s2]], [bank_b[zb]], stop=True)
            if not st["last"]:
                mm(banks[RB][:, c0:512], ones[:, :], spb[s2][:, c0:512], st["si"] == 0,
                   [cbuf, spb_b[s2]], [bank_b[RB]], stop=False)
                n2 = (st["idx"] + 1) % 2
                vop("dve", "tensor_copy", [bank_b[RB]], [rrow_b[n2]], out=rrow[n2][:, c0:512],
                    in_=banks[RB][:, c0:512])

        def emit_A3(st):
            j, c0 = geom(st)
            zb = ZB[st["idx"] % 3]
            s2 = st["idx"] % 2
            act(ptb[s2][:, c0:512], banks[zb][:, c0:512], AF.Exp, [bank_b[zb]], [ptb_b[s2]])

        def emit_PV(st):
            j, c0 = geom(st)
            s2 = st["idx"] % 2
            hh, qt, kt = st["hh"], st["qt"], st["kt"]
            ob = OB[st["g"] % 3]
            M = 64 if G == 0 else 65
            mm(banks[ob][0:M, c0:512], vt[:, kt, hh * 65:hh * 65 + M], ptb[s2][:, c0:512],
               st["si"] == 0, [vt_b, ptb_b[s2]], [bank_b[ob]], stop=st["last"])
            if st["last"]:
                if G == 0:
                    pending.append([3, "fin", st, ob])
                else:
                    pending.append([1, "ln", st, ob])

        def emit_evac(st, ob):
            hh, qt = st["hh"], st["qt"]
            op_i = qt % 2
            cols = slice(qt * 512, (qt + 1) * 512)
            if hh == 0:
                dst, dst_b = opair[op_i][0:64, :], opair_b[op_i]
            else:
                dst, dst_b = ostage[qt % 2][:, :], ostage_b[qt % 2]
            if G == 0:
                copy_any(qt + hh, dst, banks[ob][0:64, :], [bank_b[ob]], [dst_b])
            else:
                act(bcsb, banks[MB][0:64, :], AF.Exp, [bank_b[MB]], [bcsb_b], scale=-1.0)
                vop("dve", "tensor_tensor", [bank_b[ob], bcsb_b], [dst_b], out=dst,
                    in0=banks[ob][0:64, :], in1=bcsb, op=ALU.mult)
            if hh == 1:
                dma("sp", opair[op_i][64:128, :], ostage[qt % 2][:, :], [ostage_b[qt % 2]],
                    [opair_b[op_i]])
                vop("pool", "tensor_tensor", [opair_b[op_i]], [osq_b], out=osq[:, :],
                    in0=opair[op_i][:, :], in1=opair[op_i][:, :], op=ALU.mult)
                pending.append([6, "epi", st, ob])

        def emit_epi(st):
            qt = st["qt"]
            op_i = qt % 2
            cols = slice(qt * 512, (qt + 1) * 512)
            sbk = MB if G == 0 else RB
            for tt in range(4):
                mm(banks[sbk][:, 508 + tt:509 + tt], osq[:, tt * 128:(tt + 1) * 128],
                   ones[:, 0:1], True, [osq_b, cbuf], [bank_b[sbk]])
            vop("dve", "tensor_copy", [bank_b[sbk]], [ssq_b], out=ssq[:, pr, qt * 4:(qt + 1) * 4],
                in_=banks[sbk][:, 508:512])
            vop("dve", "tensor_tensor", [opair_b[op_i], gz_b[pr][qt]], [gz_b[pr][qt]],
                out=gz[:, pr, cols], in0=opair[op_i][:, :], in1=gz[:, pr, cols], op=ALU.mult)

        pending = []

        def run_stage(kind, st, ob):
            ri = st["g"] % 3
            if kind == "ln":
                act(rden[ri][64:65, :], banks[ob][64:65, :], AF.Ln, [bank_b[ob]], [rden_b[ri]])
                pending.append([2, "bc", st, ob])
            elif kind == "bc":
                mm(banks[MB][0:64, :], onesf[64:65, 0:64], rden[ri][64:65, :], True,
                   [cbuf, rden_b[ri]], [bank_b[MB]])
                pending.append([2, "fin", st, ob])
            elif kind == "epi":
                emit_epi(st)
            else:
                emit_evac(st, ob)

        def flush(all_=False):
            while True:
                for pe_ in list(pending):
                    pe_[0] -= 1
                    if pe_[0] <= 0 or all_:
                        pending.remove(pe_)
                        run_stage(pe_[1], pe_[2], pe_[3])
                if not (all_ and pending):
                    break

        if G == 0:
            emit_Z(steps[0])
            for i in range(n + 2):
                if 0 <= i - 1 < n:
                    emit_tri(steps[i - 1])
                if 0 <= i - 2 < n:
                    emit_PV(steps[i - 2])
                if i + 1 < n:
                    emit_Z(steps[i + 1])
                if i < n:
                    emit_A1(steps[i])
                if 0 <= i - 1 < n:
                    emit_A3(steps[i - 1])
                if i < n:
                    emit_A2(steps[i])
                flush()
        else:
            emit_Z(steps[0])
            if n > 1:
                emit_Z(steps[1])
            for i in range(n + 1):
                if i < n:
                    emit_A3(steps[i])
                if 0 <= i - 1 < n:
                    emit_PV(steps[i - 1])
                if i + 2 < n:
                    emit_Z(steps[i + 2])
                flush()
        flush(True)

    npairs = 8 if stop_after is None else int(stop_after[1:]) if stop_after.startswith("B") else 8
    load_w(0)
    for pr in range(npairs):
        if pr == 4:
            P.alias(mb_only, sb_only)
        in_proj(pr)
        if pr + 1 < 8:
            load_w(pr + 1)
        if pr // 4 == 1:
            for hh in range(2):
                moba_gate((pr % 4) * 2 + hh, T[hh], T_b[hh], T[2 + hh], T_b[2 + hh])
        if "qk" in dbg and pr == int(next(iter(d for d in dbg if d.startswith("pair="))).split("=")[1]):
            for i in range(2 if pr < 4 else 4):
                dump("T%d" % i, T[i][:] if pr < 4 else T[i][0:84, :], [128 if pr < 4 else 84, S], BF16,
                     [T_b[i]])
            dump("vt", vt[:], [128, NT, 130], BF16, [vt_b])
        attention(pr)
    dump("gz", gz[:, 0:npairs, :], [128, npairs, S], BF16, [b for l in gz_b for b in l])
    dump("ssq", ssq[:, 0:npairs, :], [128, npairs, NT], F32, [ssq_b])
    phaseB_bufs = (hT_b + T_b + [vt_b, gconst_b, ks_b, osq_b, wbuf_b[0], ostage_b[0]] + ptb_b + opair_b
                   + sb_only + mb_only)
    stB.close()
    if stop_after is not None and stop_after.startswith("B"):
        P.emit(nc)
        return nc, dumps

    stC = ExitStack()
    wo = sb("wo", [128, 8, D], BF16, stC)
    wg = sb("wg", [128, 8, D], BF16, stC)
    wp = sb("wp", [128, 2, D], BF16, stC)
    wo_b, wg_b, wp_b = Buf("wo"), Buf("wg"), Buf("wp")
    wst = [sb("wst%d" % i, [128, D], F32, stC) for i in range(2)]
    wst_b = [Buf("wst%d" % i) for i in range(2)]
    gfin = sb("gfin", [128, D], F32, stC)
    gfin_b = Buf("gfin")

    def mk(name, n, shape, dt):
        return ([sb("%s%d" % (name, i), shape, dt, stC) for i in range(n)],
                [Buf("%s%d" % (name, i)) for i in range(n)])

    xc, xc_b = mk("xc", 3, [128, D], F32)
    pc, pc_b = mk("pc", 3, [128, DPLE], F32)
    pcb, pcb_b = mk("pcb", 2, [128, DPLE], BF16)
    pT_sb, pT_b = mk("pTsb", 2, [128, 2, 128], BF16)
    x1, x1_b = mk("x1_", 3, [128, D], F32)
    x1b, x1b_b = mk("x1b_", 2, [128, D], BF16)
    x1T, x1T_b = mk("x1T_", 2, [128, 8, 128], BF16)
    gate, gate_b = mk("gate", 2, [128, D], F32)
    yo, yo_b = mk("yo", 2, [128, D], F32)
    stt, stt_b = mk("stt", 3, [128, 8], F32)
    junkc = sb("junkc", [128, D], BF16, stC)
    junkc_b = Buf("junkc")
    newC = ([wo_b, wg_b, wp_b, gfin_b, junkc_b] + wst_b + xc_b + pc_b + pcb_b + pT_b + x1_b + x1b_b
            + x1T_b + gate_b + yo_b + stt_b)
    P.alias(newC, phaseB_bufs)

    dma("sp", gfin[:], gfin_d, [], [gfin_b])
    w_out_v = w_out_d.rearrange("(c p) n -> p c n", p=128)
    w_gate_v = w_gate_d.rearrange("(c p) n -> p c n", p=128)
    k = 0
    for (wv, wt, wt_b, gv) in ((w_out_v, wo, wo_b, gout), (w_gate_v, wg, wg_b, gple)):
        for c in range(8):
            s = k % 2
            dma("sp", wst[s][:], wv[:, c, :], [], [wst_b[s]])
            eng = "dve" if k % 2 == 0 else "act"
            if eng == "dve":
                vop("dve", "tensor_scalar", [wst_b[s], cbuf], [wt_b], out=wt[:, c, :], in0=wst[s][:],
                    scalar1=gv[:, c:c + 1], scalar2=None, op0=ALU.mult)
            else:
                act(wt[:, c, :], wst[s][:], AF.Copy, [wst_b[s], cbuf], [wt_b], scale=gv[:, c:c + 1])
            k += 1
    dma("pool", wp[:], w_ple_d.rearrange("(c p) n -> p c n", p=128), [], [wp_b])

    rg = sb("rg", [128, NT, 2], F32, stC)
    rg_b = Buf("rg")
    P.alias([rg_b], phaseB_bufs)
    vop("dve", "tensor_reduce", [ssq_b], [rg_b], out=rg[:],
        in_=ssq[:].rearrange("p (g c) t -> p t g c", g=2), axis=AX.X, op=ALU.add)
    rstd_op(rg[:].rearrange("p t g -> p (t g)"), rg[:].rearrange("p t g -> p (t g)"), 1.0 / 512, rg_b)

    def LOAD(i):
        s3 = i % 3
        tok = slice(i * 128, (i + 1) * 128)
        dma("sp", xc[s3][:], x_d[tok, :], [], [xc_b[s3]])
        dma("sp", pc[s3][:], p_d[tok, :], [], [pc_b[s3]])

    def OPX(i):
        s2, s3 = i % 2, i % 3
        tok = slice(i * 128, (i + 1) * 128)
        for g in range(2):
            for hf in range(2):
                b = g * 2 + hf
                for c4 in range(4):
                    c = g * 4 + c4
                    mm(banks[b][:, :], gz[:, c, tok], wo[:, c, hf * 512:(hf + 1) * 512], c4 == 0,
                       gz_b[c] + [wo_b], [bank_b[b]], stop=(c4 == 3))
        for hf in range(2):
            cs = slice(hf * 512, (hf + 1) * 512)
            vop("dve", "scalar_tensor_tensor", [bank_b[hf], rg_b, xc_b[s3]], [x1_b[s3]],
                out=x1[s3][:, cs], in0=banks[hf][:, :], scalar=rg[:, i, 0:1], in1=xc[s3][:, cs],
                op0=ALU.mult, op1=ALU.add)
            vop("dve", "scalar_tensor_tensor", [bank_b[2 + hf], rg_b, x1_b[s3]], [x1_b[s3]],
                out=x1[s3][:, cs], in0=banks[2 + hf][:, :], scalar=rg[:, i, 1:2], in1=x1[s3][:, cs],
                op0=ALU.mult, op1=ALU.add)
        vop("pool", "tensor_copy", [pc_b[s3]], [pcb_b[s2]], out=pcb[s2][:], in_=pc[s3][:])
        pT5 = bank_bf(5)
        for c in range(2):
            tr(pT5[:, c * 128:(c + 1) * 128], pcb[s2][:, c * 128:(c + 1) * 128], [pcb_b[s2]], [bank_b[5]])
        copy_any(1, pT_sb[s2][:], pT5[:, 0:256].rearrange("p (c t) -> p c t", c=2), [bank_b[5]],
                 [pT_b[s2]])

    def CST(i):
        s2, s3 = i % 2, i % 3
        P.op("act", lambda e: e.copy(out=x1b[s2][:], in_=x1[s3][:]), reads=[x1_b[s3]],
             writes=[x1b_b[s2]])
        act(junkc[:], x1[s3][:], AF.Square, [x1_b[s3]], [junkc_b, stt_b[s3]],
            accum_out=stt[s3][:, 2:3])
        rstd_op(stt[s3][:, 3:4], stt[s3][:, 2:3], 1.0 / D, stt_b[s3])

    def TRE(i):
        s2, s3 = i % 2, i % 3
        pT = bank_bf(4)
        for c in range(8):
            tr(pT[:, c * 128:(c + 1) * 128], x1b[s2][:, c * 128:(c + 1) * 128], [x1b_b[s2]], [bank_b[4]])
        copy_any(0, x1T[s2][:], pT.rearrange("p (c t) -> p c t", c=8), [bank_b[4]], [x1T_b[s2]])

    def S2(i):
        s2, s3 = i % 2, i % 3
        for hf in range(2):
            cs = slice(hf * 512, (hf + 1) * 512)
            gb = 6 if hf == 0 else 5
            for c in range(8):
                mm(banks[gb][:, :], x1T[s2][:, c, :], wg[:, c, cs], c == 0, [x1T_b[s2], wg_b],
                   [bank_b[gb]], stop=(c == 7))
            act(gate[s2][:, cs], banks[gb][:, :], AF.Sigmoid, [bank_b[gb], stt_b[s3]], [gate_b[s2]],
                scale=stt[s3][:, 3:4])
            for c in range(2):
                mm(banks[7][:, :], pT_sb[s2][:, c, :], wp[:, c, cs], c == 0, [pT_b[s2], wp_b],
                   [bank_b[7]], stop=(c == 1))
            vop("dve", "tensor_tensor", [bank_b[7], gate_b[s2]], [gate_b[s2]], out=gate[s2][:, cs],
                in0=banks[7][:, :], in1=gate[s2][:, cs], op=ALU.mult)
            vop("pool", "tensor_tensor", [gate_b[s2], x1_b[s3]], [x1_b[s3]], out=x1[s3][:, cs],
                in0=gate[s2][:, cs], in1=x1[s3][:, cs], op=ALU.add)

    def S3a(i):
        s3 = i % 3
        act(junkc[:], x1[s3][:], AF.Square, [x1_b[s3]], [junkc_b, stt_b[s3]],
            accum_out=stt[s3][:, 4:5])

    def S3b(i):
        s2, s3 = i % 2, i % 3
        tok = slice(i * 128, (i + 1) * 128)
        rstd_op(stt[s3][:, 5:6], stt[s3][:, 4:5], 1.0 / D, stt_b[s3])
        vop("dve", "scalar_tensor_tensor", [x1_b[s3], stt_b[s3], gfin_b], [yo_b[s2]], out=yo[s2][:],
            in0=x1[s3][:], scalar=stt[s3][:, 5:6], in1=gfin[:], op0=ALU.mult, op1=ALU.mult)
        dma("sp", out_d[tok, :], yo[s2][:], [yo_b[s2]], [])

    LOAD(0)
    if NT > 1:
        LOAD(1)
    for t in range(-1, NT + 1):
        if 0 <= t + 3 < NT:
            LOAD(t + 3)
        if 0 <= t < NT:
            TRE(t)
        if 0 <= t + 1 < NT:
            OPX(t + 1)
        if 0 <= t - 1 < NT:
            S3a(t - 1)
        if 0 <= t < NT:
            S2(t)
        if 0 <= t + 1 < NT:
            CST(t + 1)
        if 0 <= t - 1 < NT:
            S3b(t - 1)
    stC.close()
    top.close()
    P.emit(nc)
    return nc, dumps


def _host_inputs(S, x, p, w_in, g_mix, g_out_sb, g_out_mb, w_out, w_ple, g_ple, w_ple_gate, g_final):
    c = _consts(S)
    f = np.float32
    shared = {
        "w_in": np.ascontiguousarray(w_in[0], f),
        "w_out": np.ascontiguousarray(w_out[0], f),
        "w_ple": np.ascontiguousarray(w_ple[0], f),
        "w_gate": np.ascontiguousarray(w_ple_gate[0], f),
        "gmix_bc": np.ascontiguousarray(np.broadcast_to(np.asarray(g_mix[0], f)[None, :], (128, D))),
        "gfin_bc": np.ascontiguousarray(np.broadcast_to(np.asarray(g_final, f)[None, :], (128, D))),
        "gout_pc": np.ascontiguousarray(
            np.concatenate([np.asarray(g_out_sb[0], f), np.asarray(g_out_mb[0], f)]).reshape(8, 128).T),
        "gple_pc": np.ascontiguousarray(np.asarray(g_ple[0], f).reshape(8, 128).T),
    }
    shared.update(c)
    maps = []
    for b in range(x.shape[0]):
        m = dict(shared)
        m["x"] = np.ascontiguousarray(x[b], f)
        m["p"] = np.ascontiguousarray(p[0, b], f)
        maps.append(m)
    return maps


_CACHE = {}


def kernel(x, p, w_in, g_mix, g_out_sb, g_out_mb, w_out, w_ple, g_ple, w_ple_gate, g_final):
    x = np.asarray(x)
    B, S, _ = x.shape
    if S not in _CACHE:
        _CACHE[S] = build(S)[0]
    nc = _CACHE[S]
    maps = _host_inputs(S, x, np.asarray(p), np.asarray(w_in), np.asarray(g_mix), np.asarray(g_out_sb),
                        np.asarray(g_out_mb), np.asarray(w_out), np.asarray(w_ple), np.asarray(g_ple),
                        np.asarray(w_ple_gate), np.asarray(g_final))
    res = run_bass_kernel_spmd(nc, maps, core_ids=list(range(B)))
    return np.stack([np.asarray(r["out"], np.float32) for r in res.results], axis=0)
```
